# Optimizing a Trainium2 kernel written in Bass

```python
import jax, jax.numpy as jnp
from jax import lax
import numpy as np

D_MODEL = 2048
BATCH = 8
SEQ = 2048
DEPTH = 4

N_HEADS = 16
HEAD_DIM = D_MODEL // N_HEADS
CONV_WIDTH = 31
D_FF = ((8 * D_MODEL // 3 + 255) // 256) * 256
N_EXPERTS = 8
TOP_K = 2
D_FF_EXPERT = D_FF
Q_BLOCK = 128
N_A_LAYERS = DEPTH // 2
N_B_LAYERS = DEPTH - N_A_LAYERS
N_DENSE_LAYERS = (DEPTH + 1) // 2
N_MOE_LAYERS = DEPTH // 2
EPS = 1e-6

kernel_name = "yoco_conformer_fox_moe_adaln"


def _rms_norm(x, g):
    xf = x.astype(jnp.float32)
    y = xf * lax.rsqrt(jnp.mean(xf * xf, axis=-1, keepdims=True) + EPS)
    return (y * g.astype(jnp.float32)).astype(x.dtype)


def _layer_norm(x, g, b):
    xf = x.astype(jnp.float32)
    mu = jnp.mean(xf, axis=-1, keepdims=True)
    var = jnp.mean(jnp.square(xf - mu), axis=-1, keepdims=True)
    y = (xf - mu) * lax.rsqrt(var + EPS) * g.astype(jnp.float32) + b.astype(jnp.float32)
    return y.astype(x.dtype)


def _modulate(h, shift, scale):
    return h * (1 + scale[:, None, :]) + shift[:, None, :]


def _conformer_conv(h, pw1_w, pw1_b, dw_w, dw_b, ln_g, ln_b, pw2_w, pw2_b):
    d = h.shape[-1]
    a = h @ pw1_w + pw1_b
    u = a[..., :d] * jax.nn.sigmoid(a[..., d:])
    u = lax.conv_general_dilated(
        u, dw_w[:, None, :], window_strides=(1,), padding=[(CONV_WIDTH - 1, 0)],
        dimension_numbers=("NWC", "WIO", "NWC"), feature_group_count=d) + dw_b
    u = jax.nn.silu(_layer_norm(u, ln_g, ln_b))
    return u @ pw2_w + pw2_b


def _shared_kv(x, c_act, kv_ada_w, kv_ada_b, kv_norm_g, w_kvf, b_f):
    b, s, d = x.shape
    shift, scale = jnp.split(c_act @ kv_ada_w + kv_ada_b, 2, axis=-1)
    h = _modulate(_rms_norm(x, kv_norm_g), shift, scale)
    kvf = h @ w_kvf
    k = kvf[..., :d].reshape(b, s, N_HEADS, HEAD_DIM).transpose(0, 2, 1, 3)
    v = kvf[..., d:2 * d].reshape(b, s, N_HEADS, HEAD_DIM).transpose(0, 2, 1, 3)
    log_f = jax.nn.log_sigmoid((kvf[..., 2 * d:] + b_f).astype(jnp.float32))
    cum = jnp.cumsum(log_f, axis=1).transpose(0, 2, 1)
    return k, v, cum


def _forgetting_attention(h, wq, wo, k, v, cum):
    b, s, d = h.shape
    q = (h @ wq).reshape(b, s, N_HEADS, HEAD_DIM).transpose(0, 2, 1, 3)
    scale = HEAD_DIM ** -0.5
    outs = []
    for i in range(s // Q_BLOCK):
        lo, hi = i * Q_BLOCK, (i + 1) * Q_BLOCK
        logits = jnp.einsum("bhqd,bhkd->bhqk", q[:, :, lo:hi], k[:, :, :hi]).astype(jnp.float32) * scale
        logits = logits + cum[:, :, lo:hi, None] - cum[:, :, None, :hi]
        causal = (lo + jnp.arange(Q_BLOCK))[:, None] >= jnp.arange(hi)[None, :]
        logits = jnp.where(causal, logits, -jnp.inf)
        p = jax.nn.softmax(logits, axis=-1).astype(v.dtype)
        outs.append(jnp.einsum("bhqk,bhkd->bhqd", p, v[:, :, :hi]))
    o = jnp.concatenate(outs, axis=2).transpose(0, 2, 1, 3).reshape(b, s, d)
    return o @ wo


def _swiglu(h, wg, wu, wd):
    return (jax.nn.silu(h @ wg) * (h @ wu)) @ wd


def _moe_swiglu(h, router_w, router_b, wg, wu, wd):
    b, s, d = h.shape
    t = h.reshape(b * s, d)
    logits = (t @ router_w + router_b).astype(jnp.float32)
    top_v, top_i = lax.top_k(logits, TOP_K)
    top_w = jax.nn.softmax(top_v, axis=-1)
    gates = jnp.einsum("tk,tke->te", top_w, jax.nn.one_hot(top_i, N_EXPERTS, dtype=jnp.float32)).astype(h.dtype)
    y = jnp.zeros_like(t)
    for e in range(N_EXPERTS):
        y = y + gates[:, e:e + 1] * _swiglu(t, wg[e], wu[e], wd[e])
    return y.reshape(b, s, d)


def setup_inputs(seed: int = 0) -> dict:
    key = jax.random.key(seed)
    ks = iter(jax.random.split(key, 40))
    f32 = jnp.float32
    D, H, F, E, Fe = D_MODEL, N_HEADS, D_FF, N_EXPERTS, D_FF_EXPERT

    def nrm(shape, std):
        return std * jax.random.normal(next(ks), shape, f32)

    def gain(shape):
        return 1.0 + nrm(shape, 0.02)

    return {
        "x": nrm((BATCH, SEQ, D), 1.0),
        "c": nrm((BATCH, D), 1.0),
        "ada_w": nrm((DEPTH, D, 6 * D), 0.5 * D ** -0.5),
        "ada_b": nrm((DEPTH, 6 * D), 0.02),
        "norm_mix_g": gain((DEPTH, D)),
        "norm_ffn_g": gain((DEPTH, D)),
        "conv_pw1_w": nrm((N_A_LAYERS, D, 2 * D), D ** -0.5),
        "conv_pw1_b": nrm((N_A_LAYERS, 2 * D), 0.02),
        "conv_dw_w": nrm((N_A_LAYERS, CONV_WIDTH, D), CONV_WIDTH ** -0.5),
        "conv_dw_b": nrm((N_A_LAYERS, D), 0.02),
        "conv_ln_g": gain((N_A_LAYERS, D)),
        "conv_ln_b": nrm((N_A_LAYERS, D), 0.02),
        "conv_pw2_w": nrm((N_A_LAYERS, D, D), D ** -0.5),
        "conv_pw2_b": nrm((N_A_LAYERS, D), 0.02),
        "kv_ada_w": nrm((D, 2 * D), 0.5 * D ** -0.5),
        "kv_ada_b": nrm((2 * D,), 0.02),
        "kv_norm_g": gain((D,)),
        "w_kvf": nrm((D, 2 * D + H), D ** -0.5),
        "b_f": 2.0 + 3.0 * jax.random.uniform(next(ks), (H,), f32),
        "attn_wq": nrm((N_B_LAYERS, D, D), D ** -0.5),
        "attn_wo": nrm((N_B_LAYERS, D, D), D ** -0.5),
        "ffn_w_gate": nrm((N_DENSE_LAYERS, D, F), D ** -0.5),
        "ffn_w_up": nrm((N_DENSE_LAYERS, D, F), D ** -0.5),
        "ffn_w_down": nrm((N_DENSE_LAYERS, F, D), F ** -0.5),
        "moe_router_w": nrm((N_MOE_LAYERS, D, E), D ** -0.5),
        "moe_router_b": nrm((N_MOE_LAYERS, E), 0.01),
        "moe_w_gate": nrm((N_MOE_LAYERS, E, D, Fe), D ** -0.5),
        "moe_w_up": nrm((N_MOE_LAYERS, E, D, Fe), D ** -0.5),
        "moe_w_down": nrm((N_MOE_LAYERS, E, Fe, D), Fe ** -0.5),
        "final_norm_g": gain((D,)),
    }


def reference(x, c, ada_w, ada_b, norm_mix_g, norm_ffn_g,
              conv_pw1_w, conv_pw1_b, conv_dw_w, conv_dw_b, conv_ln_g, conv_ln_b, conv_pw2_w, conv_pw2_b,
              kv_ada_w, kv_ada_b, kv_norm_g, w_kvf, b_f,
              attn_wq, attn_wo,
              ffn_w_gate, ffn_w_up, ffn_w_down,
              moe_router_w, moe_router_b, moe_w_gate, moe_w_up, moe_w_down,
              final_norm_g):
    c_act = jax.nn.silu(c)
    k_sh = v_sh = cum_sh = None
    for layer in range(DEPTH):
        if layer == N_A_LAYERS:
            k_sh, v_sh, cum_sh = _shared_kv(x, c_act, kv_ada_w, kv_ada_b, kv_norm_g, w_kvf, b_f)
        mod = c_act @ ada_w[layer] + ada_b[layer]
        sh1, sc1, g1, sh2, sc2, g2 = jnp.split(mod, 6, axis=-1)

        h = _modulate(_rms_norm(x, norm_mix_g[layer]), sh1, sc1)
        if layer < N_A_LAYERS:
            i = layer
            y = _conformer_conv(h, conv_pw1_w[i], conv_pw1_b[i], conv_dw_w[i], conv_dw_b[i],
                                conv_ln_g[i], conv_ln_b[i], conv_pw2_w[i], conv_pw2_b[i])
        else:
            i = layer - N_A_LAYERS
            y = _forgetting_attention(h, attn_wq[i], attn_wo[i], k_sh, v_sh, cum_sh)
        x = x + g1[:, None, :] * y

        h = _modulate(_rms_norm(x, norm_ffn_g[layer]), sh2, sc2)
        j = layer // 2
        if layer % 2 == 0:
            y = _swiglu(h, ffn_w_gate[j], ffn_w_up[j], ffn_w_down[j])
        else:
            y = _moe_swiglu(h, moe_router_w[j], moe_router_b[j], moe_w_gate[j], moe_w_up[j], moe_w_down[j])
        x = x + g2[:, None, :] * y
    return _rms_norm(x, final_norm_g)
```

```python
import contextlib
import numpy as np
import concourse.bass as bass
import concourse.mybir as mybir
from concourse.bass_utils import run_bass_kernel_spmd

F32, BF16 = mybir.dt.float32, mybir.dt.bfloat16
AF = mybir.ActivationFunctionType
ALU = mybir.AluOpType
AX = mybir.AxisListType

D = 2048; S = 2048; H = 16; HD = 128; CW = 31; DFF = 5632; NE = 8
KC = D // 128; FC = DFF // 128
EPS = 1e-6
NCORES = 8
WSLOT = FC * 128

VEC_NAMES = ["c"]
for _l in range(4):
    VEC_NAMES += [f"ab{_l}_{s}" for s in range(6)]
VEC_NAMES += ["kvab_0", "kvab_1"]
for _l in range(4):
    VEC_NAMES += [f"nmix{_l}", f"nffn{_l}"]
for _i in range(2):
    VEC_NAMES += [f"pw1bv{_i}", f"pw1bg{_i}", f"dwb{_i}", f"lng{_i}", f"lnb{_i}", f"pw2b{_i}"]
VEC_NAMES += ["kvng", "fng"]
VIDX = {n: i for i, n in enumerate(VEC_NAMES)}
NV = len(VEC_NAMES)


def _fm(v):
    return np.ascontiguousarray(np.asarray(v, np.float32).reshape(KC, 128).T)


class Sem:
    def __init__(self, h):
        self.h = h
        self.total = 0


class Eng:
    def __init__(self, k, e, name):
        self.k = k; self.e = e; self.name = name
        self.sem = k.newsem("p_" + name)
        self.seen = {}

    def wait(self, *deps):
        for d in deps:
            if d is None:
                continue
            if isinstance(d, list):
                self.wait(*d)
                continue
            sem, v = d
            if self.seen.get(id(sem), 0) >= v:
                continue
            self.e.wait_ge(sem.h, v)
            self.seen[id(sem)] = v

    def mark(self, ins):
        ins.then_inc(self.sem.h, 1)
        self.sem.total += 1
        return (self.sem, self.sem.total)

    def dma(self, out, in_, dsem):
        ins = self.e.dma_start(out=out, in_=in_)
        ins.then_inc(dsem.h, 16)
        dsem.total += 16
        return (dsem, dsem.total)


class Ring:
    def __init__(self, k, name, views, dma=True, es=None):
        self.views = views; self.n = len(views)
        self.dsems = [k.newsem(f"{name}{i}", es) for i in range(self.n)] if dma else None
        self.free = [None] * self.n
        self.i = 0

    def next(self):
        s = self.i % self.n
        self.i += 1
        return s


class K:
    def __init__(self):
        self.nc = bass.Bass("TRN2", target_bir_lowering=False)
        self.es = contextlib.ExitStack()
        self.nsem = 0

    def newsem(self, name, es=None):
        pool = self.__dict__.setdefault("sempool", [])
        if es is not None and pool:
            sem = pool.pop()
        else:
            self.nsem += 1
            sem = Sem(self.es.enter_context(self.nc.semaphore(f"{name}_{self.nsem}")))
        if es is not None:
            es.callback(pool.append, sem)
        return sem

    def sb(self, name, shape, dt, es=None):
        self.nsb = getattr(self, "nsb", 0) + 1
        return (es or self.es).enter_context(self.nc.sbuf_tensor(f"{name}_s{self.nsb}", list(shape), dt))


def build(stop_after=None, dbg=None, nseq=1):
    k = K()
    nc = k.nc
    with k.es:
        _build(k, nc, stop_after, dbg, nseq)
    return nc


def _build(k, nc, stop_after, dbg, nseq):
    def din(name, shape, dt=F32):
        return nc.dram_tensor(name, list(shape), dt, kind="ExternalInput").ap()

    xT_in = din("xT", [nseq, D, S])
    vecs_d = din("vecs", [nseq, 128, NV * 16])
    cur = [0]
    dww_d = din("dww", [2, 128, CW * 16])
    bf_d = din("bf", [16, 1])
    rw_d = din("rw", [2, 128, KC * NE])
    rb_d = din("rb", [2, NE, 1])
    ident_d = din("ident", [128, 128])
    tri_d = din("tri", [128, 128])
    WSHAPES = dict(ada_w=[4, D, 6 * D], conv_pw1_w=[2, D, 2 * D], conv_pw2_w=[2, D, D], kv_ada_w=[D, 2 * D],
                   w_kvf=[D, 2 * D + H], attn_wq=[2, D, D], attn_wo=[2, D, D], ffn_w_gate=[2, D, DFF],
                   ffn_w_up=[2, D, DFF], ffn_w_down=[2, DFF, D], moe_w_gate=[2, NE, D, DFF],
                   moe_w_up=[2, NE, D, DFF], moe_w_down=[2, NE, DFF, D])
    wcache = {}

    def WT(name):
        if name not in wcache:
            wcache[name] = din(name, WSHAPES[name])
        return wcache[name]
    outT_all = nc.dram_tensor("outT", [nseq, D, S], F32, kind="ExternalOutput").ap()
    dbg_out = None
    if dbg is not None:
        dbg_out = nc.dram_tensor("dbg", list(dbg), F32, kind="ExternalOutput").ap()
    xTs = nc.dram_tensor("xTs", [D, S], F32, kind="Internal").ap()
    KTs = nc.dram_tensor("KTs", [D, S], BF16, kind="Internal").ap()
    Vs = nc.dram_tensor("Vs", [S, D], BF16, kind="Internal").ap()

    PE = Eng(k, nc.tensor, "pe"); ACT = Eng(k, nc.scalar, "act"); DVE = Eng(k, nc.vector, "dve")
    POOL = Eng(k, nc.gpsimd, "pool"); SP = Eng(k, nc.sync, "sp")
    bar = k.newsem("bar")
    BAR_ENGS = [PE, ACT, DVE, SP]

    def barrier():
        for e in BAR_ENGS:
            e.e.drain().then_inc(bar.h, 1)
        bar.total += len(BAR_ENGS)
        for e in BAR_ENGS:
            e.e.wait_ge(bar.h, bar.total)

    vecs = k.sb("vecs", [128, NV * 16], F32)
    modall = k.sb("modall", [128, 4 * 96 + 32], F32)
    ident_f = k.sb("ident_f", [128, 128], F32)
    ident_b = k.sb("ident_b", [128, 128], BF16)
    tri_b = k.sb("tri_b", [128, 128], BF16)
    ones_b = k.sb("ones_b", [128, 128], BF16)
    cact = k.sb("cact", [128, KC], BF16)
    eps_c = k.sb("eps_c", [128, 1], F32)
    deps_c = k.sb("deps_c", [128, 1], F32)
    negF = k.sb("negF", [128, 16 * H], F32)
    GqT = k.sb("GqT", [H, S], BF16)
    wring_t = k.sb("wring", [128, 3 * WSLOT], BF16)
    wring = Ring(k, "w", [wring_t[:, i * WSLOT:(i + 1) * WSLOT] for i in range(3)])
    psb = [k.es.enter_context(nc.psum_tensor(f"ps{i}", [128, 512], F32)) for i in range(8)]
    bank_free = [None] * 8
    bank_i = [0]
    misc = k.newsem("misc")

    def V(name):
        i = VIDX[name]
        return vecs[:, i * 16:(i + 1) * 16]

    def MOD(l, s):
        return modall[:, l * 96 + s * 16: l * 96 + (s + 1) * 16]

    def KVMOD(s):
        return modall[:, 384 + s * 16: 384 + (s + 1) * 16]

    def bank_acquire():
        b = bank_i[0] % 8
        bank_i[0] += 1
        PE.wait(bank_free[b])
        return b

    def selfwait(E, dep):
        E.wait(dep)

    d0 = SP.dma(ident_f[:], ident_d[:, :], misc)
    tri_f = k.sb("tri_f", [128, 128], F32)
    d0 = SP.dma(tri_f[:], tri_d[:, :], misc)
    DVE.wait(d0)
    DVE.e.tensor_copy(out=ident_b[:], in_=ident_f[:])
    DVE.e.tensor_copy(out=tri_b[:], in_=tri_f[:])
    DVE.e.memset(eps_c[:], float(EPS))
    DVE.e.memset(deps_c[:], float(D * EPS))
    m = DVE.mark(DVE.e.memset(ones_b[:], 1.0))
    PE.wait(m); ACT.wait(m); SP.wait(m)
    barrier()

    def seq_setup():
        dv = SP.dma(vecs[:], vecs_d[cur[0]], misc)
        dx = SP.dma(xTs[:, :], xT_in[cur[0]], misc)
        ACT.wait(dv)
        cdep = ACT.mark(ACT.e.activation(out=cact[:], in_=V("c"), func=AF.Silu))
        SP.wait(dx)
        PE.wait(cdep); DVE.wait(dv)
        barrier()

    def phase_mod():
        srcs = [(WT("ada_w")[l], 96, l * 96, VIDX[f"ab{l}_0"]) for l in range(4)] + [(WT("kv_ada_w"), 32, 384, VIDX["kvab_0"])]
        for (w, nch, off, vi) in srcs:
            b = bank_acquire()
            for j2 in range(nch // 2):
                s = wring.next()
                POOL.wait(wring.free[s])
                sv = wring.views[s][:, 0:KC * 256].rearrange("p (kc n) -> p kc n", kc=KC)
                dep = POOL.dma(sv, w[:, j2 * 256:(j2 + 1) * 256].rearrange("(kc p) n -> p kc n", p=128), wring.dsems[s])
                PE.wait(dep)
                for jj in range(2):
                    j = j2 * 2 + jj
                    for kc in range(KC):
                        ins = PE.e.matmul(psb[b][:, j:j + 1], lhsT=sv[:, kc, jj * 128:(jj + 1) * 128], rhs=cact[:, kc:kc + 1],
                                          start=(kc == 0), stop=(kc == KC - 1))
                wring.free[s] = PE.mark(ins)
            rdy = wring.free[s]
            DVE.wait(rdy)
            bank_free[b] = DVE.mark(DVE.e.tensor_tensor(out=modall[:, off:off + nch], in0=psb[b][:, 0:nch],
                                                        in1=vecs[:, vi * 16: vi * 16 + nch], op=ALU.add))


    def phase_norm(ph, g_ap, sh_ap, sc_ap, hT, tok0, ntok, router=None, out_dram=None, scratch=None):
        off = [0]

        def alloc(name, shape, dt):
            if scratch is None:
                return k.sb(name, shape, dt, ph)
            n = int(np.prod(shape[1:])) * (2 if dt == F32 else 1)
            v = scratch[:, off[0]:off[0] + n]
            off[0] += n
            if dt == F32:
                v = v.bitcast(F32)
            if len(shape) == 3:
                v = v.rearrange("p (a b) -> p a b", a=shape[1])
            return v
        avec = k.sb("avec", [128, KC], F32, ph)
        xt = [alloc(f"xt{i}", [128, KC, 512], F32) for i in range(2)]
        xring = Ring(k, "nx", [t[:] for t in xt], es=ph)
        sq = [alloc(f"sq{i}", [128, 4, 512], BF16) for i in range(2)]
        sq_free = [None, None]
        rstd = [alloc(f"rstd{i}", [128, 512], F32) for i in range(2)]
        tmp = [alloc(f"ntmp{i}", [128, 512], F32) for i in range(3)]
        tmp_free = [None] * 3
        rstd_free = [None, None]
        fin_sems = [k.newsem(f"fin{q}", ph) for q in range(3)] if out_dram is not None else None
        a0 = DVE.mark(DVE.e.scalar_tensor_tensor(out=avec[:], in0=sc_ap, scalar=1.0, in1=g_ap, op0=ALU.add, op1=ALU.mult))
        DVE.wait(a0)
        adep = DVE.mark(DVE.e.tensor_scalar(out=avec[:], in0=avec[:], scalar1=float(np.sqrt(D)), scalar2=0.0, op0=ALU.mult, op1=ALU.add))
        DVE.wait(adep)
        nt = ntok // 512
        ti = 0
        last = None
        for tb in range(nt):
            s = xring.next()
            SP.wait(xring.free[s])
            xdep = SP.dma(xring.views[s], xTs[:, tok0 + tb * 512: tok0 + (tb + 1) * 512].rearrange("(c p) t -> p c t", p=128),
                          xring.dsems[s])
            x = xt[s]
            b = bank_acquire()
            ACT.wait(xdep)
            for g4 in range(4):
                q = (tb * 4 + g4) % 2
                ACT.wait(sq_free[q])
                sdep = ACT.mark(ACT.e.activation(out=sq[q][:], in_=x[:, g4 * 4:(g4 + 1) * 4, :], func=AF.Square))
                PE.wait(sdep)
                for c4 in range(4):
                    c = g4 * 4 + c4
                    ins = PE.e.matmul(psb[b][:, :], lhsT=ones_b[:], rhs=sq[q][:, c4, :], start=(c == 0), stop=(c == KC - 1))
                sq_free[q] = PE.mark(ins)
            r = rstd[tb % 2]
            ACT.wait(sq_free[q], rstd_free[tb % 2])
            r0 = ACT.mark(ACT.e.activation(out=r[:], in_=psb[b][:, :], func=AF.Sqrt, bias=deps_c[:, 0:1], scale=1.0))
            bank_free[b] = r0
            DVE.wait(r0)
            rdep = DVE.mark(DVE.e.reciprocal(out=r[:], in_=r[:]))
            DVE.wait(rdep, xdep)
            rb_bank = None
            if router is not None:
                rb_bank = bank_acquire()
            for c in range(KC):
                t = tmp[ti % 3]
                DVE.wait(tmp_free[ti % 3])
                tdep = DVE.mark(DVE.e.scalar_tensor_tensor(out=t[:], in0=x[:, c, :], scalar=avec[:, c:c + 1], in1=r[:],
                                                           op0=ALU.mult, op1=ALU.mult))
                if out_dram is not None:
                    SP.wait(tdep)
                    fdep = SP.dma(out_dram[c * 128:(c + 1) * 128, tok0 + tb * 512: tok0 + (tb + 1) * 512], t[:], fin_sems[ti % 3])
                    tmp_free[ti % 3] = fdep
                    ti += 1
                    last = fdep
                    continue
                ACT.wait(tdep)
                fdep = ACT.mark(ACT.e.activation(out=hT[:, c, tb * 512:(tb + 1) * 512], in_=t[:], func=AF.Identity,
                                                 bias=sh_ap[:, c:c + 1], scale=1.0))
                if router is not None:
                    PE.wait(tdep)
                    ins = PE.e.matmul(psb[rb_bank][0:NE, :], lhsT=router["rw"][:, c, :], rhs=t[:], start=(c == 0), stop=(c == KC - 1))
                    pdep = PE.mark(ins)
                    tmp_free[ti % 3] = [fdep, pdep]
                else:
                    tmp_free[ti % 3] = fdep
                ti += 1
                last = fdep
            xring.free[s] = [tdep]
            rstd_free[tb % 2] = tdep
            if router is not None:
                ACT.wait(pdep)
                bank_free[rb_bank] = ACT.mark(ACT.e.activation(out=router["lgT"][:, tb * 512:(tb + 1) * 512], in_=psb[rb_bank][0:NE, :],
                                                               func=AF.Identity, bias=router["bias"][:, 0:1], scale=1.0))
        return last

    def run_gemm(steps, n_sub, kc_n, wload, rhs, tbs, epi, pre=None, ahead=2):
        steps = list(steps); tbs = list(tbs)
        its = [(t_, tb_) for t_ in steps for tb_ in tbs]
        if pre is not None:
            for i_ in range(min(ahead, len(its))):
                pre(*its[i_])
        idx = 0
        for t in steps:
            s = wring.next()
            POOL.wait(wring.free[s])
            deps, subs = wload(t, wring.views[s])
            PE.wait(deps)
            for tb in tbs:
                if pre is not None and idx + ahead < len(its):
                    pre(*its[idx + ahead])
                idx += 1
                banks = [bank_acquire() for _ in range(n_sub)]
                rdy = []
                for si in range(n_sub):
                    for kc in range(kc_n):
                        ins = PE.e.matmul(psb[banks[si]][:, :], lhsT=subs[si][:, kc, :], rhs=rhs(kc, tb),
                                          start=(kc == 0), stop=(kc == kc_n - 1))
                    rdy.append(PE.mark(ins))
                fr = epi(t, tb, banks, rdy)
                for bi, b in enumerate(banks):
                    bank_free[b] = fr[bi]
            wring.free[s] = rdy[-1]

    def wload_cols(wmat, ncols_list, kc_n):
        def f(t, slot):
            deps = []; subs = []
            cols = ncols_list(t)
            for i, c0 in enumerate(cols):
                sv = slot[:, i * kc_n * 128:(i + 1) * kc_n * 128].rearrange("p (kc n) -> p kc n", kc=kc_n)
                deps.append(POOL.dma(sv, wmat[:, c0:c0 + 128].rearrange("(kc p) n -> p kc n", p=128), wring.dsems[wring_slot_of(slot)]))
                subs.append(sv)
            return deps, subs
        return f

    slot_ids = {}
    for i in range(3):
        slot_ids[i] = wring.views[i]

    def wring_slot_of(slot):
        for i in range(3):
            if slot is wring.views[i]:
                return i
        raise KeyError

    xs_t = [k.sb(f"xs{i}", [128, 512], F32) for i in range(3)]
    xsring = Ring(k, "xs", [t[:] for t in xs_t])
    xs_store = [k.newsem(f"xst{i}") for i in range(3)]
    et_t = [k.sb(f"et{i}", [128, 512], F32) for i in range(2)]
    et_free = [None, None]
    et_i = [0]

    def make_res_epi(gvec, gbvec, tokbase, gate_b=None, out_dram=None):
        state = {}
        dst = xTs if out_dram is None else out_dram

        def pre(t, tb):
            s = xsring.next()
            SP.wait(xsring.free[s])
            state[(t, tb)] = (s, SP.dma(xsring.views[s], xTs[t * 128:(t + 1) * 128, tokbase + tb * 512: tokbase + (tb + 1) * 512],
                                        xsring.dsems[s]))

        def epi(t, tb, banks, rdy):
            s, xdep = state.pop((t, tb))
            b = banks[0]
            e = et_i[0] % 2
            et_i[0] += 1
            et = et_t[e]
            if gate_b is None:
                ACT.wait(rdy[0], et_free[e])
                if gbvec is None:
                    adep = ACT.mark(ACT.e.activation(out=et[:], in_=psb[b][:, :], func=AF.Identity, scale=gvec[:, t:t + 1]))
                else:
                    adep = ACT.mark(ACT.e.activation(out=et[:], in_=psb[b][:, :], func=AF.Identity, scale=gvec[:, t:t + 1],
                                                     bias=gbvec[:, t:t + 1]))
                DVE.wait(adep, xdep)
                ddep = DVE.mark(DVE.e.tensor_tensor(out=xs_t[s][:], in0=xs_t[s][:], in1=et[:], op=ALU.add))
                et_free[e] = ddep
                fr = adep
            else:
                DVE.wait(rdy[0], et_free[e], xdep)
                a1 = DVE.mark(DVE.e.tensor_tensor(out=et[:], in0=psb[b][:, :], in1=gate_b[:, tb * 512:(tb + 1) * 512], op=ALU.mult))
                DVE.wait(a1)
                ddep = DVE.mark(DVE.e.scalar_tensor_tensor(out=xs_t[s][:], in0=et[:], scalar=gvec[:, t:t + 1], in1=xs_t[s][:],
                                                           op0=ALU.mult, op1=ALU.add))
                et_free[e] = ddep
                fr = a1
            SP.wait(ddep)
            sdep = SP.dma(dst[t * 128:(t + 1) * 128, tokbase + tb * 512: tokbase + (tb + 1) * 512], xs_t[s][:], xs_store[s])
            xsring.free[s] = sdep
            return [fr]
        return pre, epi

    def finish_stores():
        for s in range(3):
            SP.wait((xs_store[s], xs_store[s].total))

    def ffn_half(ph, hT, wg, wu, wd, gvec, tokbase, aT, s_t, gate_b=None):
        s_free = [None, None]
        si = [0]

        def wload_gu(t, slot):
            sid = wring_slot_of(slot)
            svg = slot[:, 0:KC * 128].rearrange("p (kc n) -> p kc n", kc=KC)
            svu = slot[:, KC * 128:2 * KC * 128].rearrange("p (kc n) -> p kc n", kc=KC)
            d1 = POOL.dma(svg, wg[:, t * 128:(t + 1) * 128].rearrange("(kc p) n -> p kc n", p=128), wring.dsems[sid])
            d2 = POOL.dma(svu, wu[:, t * 128:(t + 1) * 128].rearrange("(kc p) n -> p kc n", p=128), wring.dsems[sid])
            return [d1, d2], [svg, svu]

        last_a = [None]

        def epi_gu(t, tb, banks, rdy):
            q = si[0] % 2
            si[0] += 1
            ACT.wait(rdy[0], s_free[q])
            sdep = ACT.mark(ACT.e.activation(out=s_t[q][:], in_=psb[banks[0]][:, :], func=AF.Silu))
            DVE.wait(sdep, rdy[1], ffn_state.get("aT_free"))
            adep = DVE.mark(DVE.e.tensor_tensor(out=aT[:, t, tb * 512:(tb + 1) * 512], in0=psb[banks[1]][:, :], in1=s_t[q][:], op=ALU.mult))
            s_free[q] = adep
            last_a[0] = adep
            return [sdep, adep]

        run_gemm(range(FC), 2, KC, wload_gu, lambda kc, tb: hT[:, kc, tb * 512:(tb + 1) * 512], range(2), epi_gu)
        PE.wait(last_a[0])

        def wload_d(t, slot):
            sid = wring_slot_of(slot)
            sv = slot[:, 0:FC * 128].rearrange("p (kc n) -> p kc n", kc=FC)
            d1 = POOL.dma(sv, wd[:, t * 128:(t + 1) * 128].rearrange("(kc p) n -> p kc n", p=128), wring.dsems[sid])
            return [d1], [sv]

        pre, epi = make_res_epi(gvec, None, tokbase, gate_b=gate_b)
        run_gemm(range(KC), 1, FC, wload_d, lambda kc, tb: aT[:, kc, tb * 512:(tb + 1) * 512], range(2), epi, pre=pre)
        ffn_state["aT_free"] = (PE.sem, PE.sem.total)

    ffn_state = {}

    def conv_layer(l):
        i = l
        with contextlib.ExitStack() as ph:
            hT = k.sb("hT", [128, KC, S], BF16, ph)
            with contextlib.ExitStack() as ph2:
                phase_norm(ph2, V(f"nmix{l}"), MOD(l, 0), MOD(l, 1), hT, 0, S)
                barrier()
            if stop_after == f"norm_{l}":
                POOL.e.wait_ge(bar.h, bar.total)
                POOL.dma(dbg_out[:, :].rearrange("(c p) t -> p c t", p=128), hT[:, :, :], misc)
                SP.wait((misc, misc.total))
                return True
            uT = k.sb("uT", [128, KC, S + 32], BF16, ph)
            phs = contextlib.ExitStack()
            DVE.e.memset(uT[:, :, 0:30], 0.0)
            sg_t = [k.sb(f"sg{q}", [128, 512], BF16, phs) for q in range(2)]
            sg_free = [None, None]
            sgi = [0]

            def wload1(t, slot):
                sid = wring_slot_of(slot)
                svv = slot[:, 0:KC * 128].rearrange("p (kc n) -> p kc n", kc=KC)
                svg = slot[:, KC * 128:2 * KC * 128].rearrange("p (kc n) -> p kc n", kc=KC)
                d1 = POOL.dma(svv, WT("conv_pw1_w")[i][:, t * 128:(t + 1) * 128].rearrange("(kc p) n -> p kc n", p=128), wring.dsems[sid])
                d2 = POOL.dma(svg, WT("conv_pw1_w")[i][:, D + t * 128: D + (t + 1) * 128].rearrange("(kc p) n -> p kc n", p=128), wring.dsems[sid])
                return [d1, d2], [svv, svg]

            bv = V(f"pw1bv{i}"); bg = V(f"pw1bg{i}")

            def epi1(t, tb, banks, rdy):
                q = sgi[0] % 2
                sgi[0] += 1
                ACT.wait(rdy[1], sg_free[q])
                sdep = ACT.mark(ACT.e.activation(out=sg_t[q][:], in_=psb[banks[1]][:, :], func=AF.Sigmoid, bias=bg[:, t:t + 1], scale=1.0))
                DVE.wait(sdep, rdy[0])
                udep = DVE.mark(DVE.e.scalar_tensor_tensor(out=uT[:, t, 30 + tb * 512: 30 + (tb + 1) * 512], in0=psb[banks[0]][:, :],
                                                           scalar=bv[:, t:t + 1], in1=sg_t[q][:], op0=ALU.add, op1=ALU.mult))
                sg_free[q] = udep
                return [udep, sdep]

            run_gemm(range(KC), 2, KC, wload1, lambda kc, tb: hT[:, kc, tb * 512:(tb + 1) * 512], range(4), epi1)
            barrier()
            if stop_after == f"pw1_{l}":
                POOL.e.wait_ge(bar.h, bar.total)
                POOL.dma(dbg_out[:, :].rearrange("(c p) t -> p c t", p=128), uT[:, :, 30:30 + S], misc)
                SP.wait((misc, misc.total))
                phs.close()
                return True
            phs.close(); phs = contextlib.ExitStack()
            vT = hT
            dww = k.sb("dww", [128, CW * 16], F32, phs)
            dwd = SP.dma(dww[:], dww_d[i], misc)
            diag = [k.sb(f"diag{q}", [128, CW, 128], BF16, phs) for q in range(2)]
            diag_free = [None, None]
            dwb = V(f"dwb{i}")
            DVE.wait(dwd)
            for j in range(KC):
                q = j % 2
                DVE.wait(diag_free[q])
                for kk in range(CW):
                    ins = DVE.e.tensor_scalar(out=diag[q][:, kk, :], in0=ident_b[:], scalar1=dww[:, kk * 16 + j: kk * 16 + j + 1],
                                              scalar2=0.0, op0=ALU.mult, op1=ALU.add)
                ddep = DVE.mark(ins)
                PE.wait(ddep)
                for tb in range(4):
                    b = bank_acquire()
                    for kk in range(CW):
                        ins = PE.e.matmul(psb[b][:, :], lhsT=diag[q][:, kk, :], rhs=uT[:, j, tb * 512 + kk: tb * 512 + kk + 512],
                                          start=(kk == 0), stop=(kk == CW - 1))
                    rdy = PE.mark(ins)
                    ACT.wait(rdy)
                    bank_free[b] = ACT.mark(ACT.e.activation(out=vT[:, j, tb * 512:(tb + 1) * 512], in_=psb[b][:, :], func=AF.Identity,
                                                             bias=dwb[:, j:j + 1], scale=1.0))
                diag_free[q] = rdy
            barrier()
            if stop_after == f"dw_{l}":
                POOL.e.wait_ge(bar.h, bar.total)
                POOL.dma(dbg_out[:, :].rearrange("(c p) t -> p c t", p=128), vT[:, :, :], misc)
                SP.wait((misc, misc.total))
                phs.close()
                return True
            phs.close(); phs = contextlib.ExitStack()
            sq = [k.sb(f"csq{q}", [128, 4, 512], BF16, phs) for q in range(2)]
            sq_free = [None, None]
            st = {n: k.sb(f"ln_{n}", [128, 512], F32, phs) for n in ["m", "msq", "var", "rstd"]}
            tmp = [k.sb(f"ctmp{q}", [128, 512], F32, phs) for q in range(3)]
            tmp_free = [None] * 3
            lng = V(f"lng{i}"); lnb = V(f"lnb{i}")
            ti = 0
            prev_tb_done = None
            for tb in range(4):
                b1 = bank_acquire(); b2 = bank_acquire()
                for g4 in range(4):
                    q = (tb * 4 + g4) % 2
                    ACT.wait(sq_free[q])
                    sdep = ACT.mark(ACT.e.activation(out=sq[q][:], in_=vT[:, g4 * 4:(g4 + 1) * 4, tb * 512:(tb + 1) * 512], func=AF.Square))
                    PE.wait(sdep)
                    for c4 in range(4):
                        c = g4 * 4 + c4
                        PE.e.matmul(psb[b1][:, :], lhsT=ones_b[:], rhs=vT[:, c, tb * 512:(tb + 1) * 512], start=(c == 0), stop=(c == KC - 1))
                        ins = PE.e.matmul(psb[b2][:, :], lhsT=ones_b[:], rhs=sq[q][:, c4, :], start=(c == 0), stop=(c == KC - 1))
                    sq_free[q] = PE.mark(ins)
                DVE.wait(sq_free[q], prev_tb_done)
                d1 = DVE.mark(DVE.e.tensor_scalar(out=st["m"][:], in0=psb[b1][:, :], scalar1=1.0 / D, scalar2=0.0, op0=ALU.mult, op1=ALU.add))
                DVE.wait(d1)
                d2 = DVE.mark(DVE.e.tensor_tensor(out=st["msq"][:], in0=st["m"][:], in1=st["m"][:], op=ALU.mult))
                DVE.wait(d2)
                d3 = DVE.mark(DVE.e.scalar_tensor_tensor(out=st["var"][:], in0=psb[b2][:, :], scalar=1.0 / D, in1=st["msq"][:],
                                                         op0=ALU.mult, op1=ALU.subtract))
                DVE.wait(d3)
                ACT.wait(d3)
                d4a = ACT.mark(ACT.e.activation(out=st["rstd"][:], in_=st["var"][:], func=AF.Sqrt, bias=eps_c[:, 0:1], scale=1.0))
                DVE.wait(d4a)
                d4 = DVE.mark(DVE.e.reciprocal(out=st["rstd"][:], in_=st["rstd"][:]))
                bank_free[b1] = d4; bank_free[b2] = d4
                DVE.wait(d4)
                for c in range(KC):
                    t = tmp[ti % 3]
                    DVE.wait(tmp_free[ti % 3])
                    e1 = DVE.mark(DVE.e.tensor_tensor(out=t[:], in0=vT[:, c, tb * 512:(tb + 1) * 512], in1=st["m"][:], op=ALU.subtract))
                    DVE.wait(e1)
                    e2 = DVE.mark(DVE.e.tensor_tensor(out=t[:], in0=t[:], in1=st["rstd"][:], op=ALU.mult))
                    ACT.wait(e2)
                    zdep = ACT.mark(ACT.e.activation(out=vT[:, c, tb * 512:(tb + 1) * 512], in_=t[:], func=AF.Silu,
                                                     scale=lng[:, c:c + 1], bias=lnb[:, c:c + 1]))
                    tmp_free[ti % 3] = zdep
                    ti += 1
                prev_tb_done = e2
            barrier()
            phs.close(); phs = contextlib.ExitStack()
            gb = k.sb("gb", [128, KC], F32, phs)
            gdep = DVE.mark(DVE.e.tensor_tensor(out=gb[:], in0=MOD(l, 2), in1=V(f"pw2b{i}"), op=ALU.mult))
            ACT.wait(gdep)

            def wload2(t, slot):
                sid = wring_slot_of(slot)
                sv = slot[:, 0:KC * 128].rearrange("p (kc n) -> p kc n", kc=KC)
                d1 = POOL.dma(sv, WT("conv_pw2_w")[i][:, t * 128:(t + 1) * 128].rearrange("(kc p) n -> p kc n", p=128), wring.dsems[sid])
                return [d1], [sv]

            pre, epi = make_res_epi(MOD(l, 2), gb, 0)
            run_gemm(range(KC), 1, KC, wload2, lambda kc, tb: vT[:, kc, tb * 512:(tb + 1) * 512], range(4), epi, pre=pre)
            finish_stores()
            barrier()
            phs.close()
        return False

    def ffn_layer(l):
        j = l // 2
        moe = (l % 2 == 1)
        with contextlib.ExitStack() as ph:
            hTh = k.sb("hTh", [128, KC, 1024], BF16, ph)
            aT = k.sb("aT", [128, FC, 1024], BF16, ph)
            s_t = [k.sb(f"s{q}", [128, 512], BF16, ph) for q in range(2)]
            ffn_state.clear()
            if moe:
                rw = k.sb("rw", [128, KC, NE], F32, ph)
                rbias = k.sb("rbias", [NE, 1], F32, ph)
                rb0 = k.sb("rb0", [NE, 1], F32, ph)
                shf = k.sb("shf", [128, KC], F32, ph)
                lgT = k.sb("lgT", [NE, 1024], F32, ph)
                lg = k.sb("lg", [128, 8, NE], F32, ph)
                gates = k.sb("gates", [128, 8, NE], F32, ph)
                gT = k.sb("gT", [NE, 1024], BF16, ph)
                gate_b = [k.sb(f"gate_b{q}", [128, 1024], F32, ph) for q in range(2)]
                gate_free = [None, None]
                sel8 = [k.sb(f"sel8{q}", [NE, 128], BF16, ph) for q in range(2)]
                ones8 = k.sb("ones8", [NE, 128], F32, ph)
                sm = {n: k.sb(f"rt_{n}", [128, 8], F32, ph) for n in ["m1", "m2", "d", "e", "w1", "w2"]}
                big = {n: k.sb(f"rt_{n}", [128, 8, NE], F32, ph) for n in ["eq1", "l2", "eq2", "g1"]}
                d1 = SP.dma(rw[:], rw_d[j].rearrange("p (c e) -> p c e", e=NE), misc)
                d1 = SP.dma(rb0[:], rb_d[j], misc)
                DVE.wait(d1)
                DVE.e.memset(ones8[:], 1.0)
                PE.wait(d1)
                bb = bank_acquire()
                DVE.e.tensor_copy(out=shf[:], in_=MOD(l, 3))
                sdep = DVE.mark(DVE.e.tensor_copy(out=shf[:], in_=MOD(l, 3)))
                PE.wait(sdep)
                for c in range(KC):
                    ins = PE.e.matmul(psb[bb][0:NE, 0:1], lhsT=rw[:, c, :], rhs=shf[:, c:c + 1], start=(c == 0), stop=(c == KC - 1))
                pd = PE.mark(ins)
                DVE.wait(pd)
                bank_free[bb] = DVE.mark(DVE.e.tensor_tensor(out=rbias[:], in0=psb[bb][0:NE, 0:1], in1=rb0[:], op=ALU.add))
                ACT.wait(bank_free[bb])
            for half in range(2):
                tok0 = half * 1024
                with contextlib.ExitStack() as ph2:
                    router = dict(rw=rw, lgT=lgT, bias=rbias) if moe else None
                    phase_norm(ph2, V(f"nffn{l}"), MOD(l, 3), MOD(l, 4), hTh, tok0, 1024, router=router,
                               scratch=aT[:].rearrange("p f t -> p (f t)"))
                    barrier()
                if not moe:
                    ffn_half(ph, hTh, WT("ffn_w_gate")[j], WT("ffn_w_up")[j], WT("ffn_w_down")[j], MOD(l, 5), tok0, aT, s_t)
                else:
                    b = bank_acquire()
                    for tl in range(8):
                        ins = PE.e.transpose(out=psb[b][:, tl * NE:(tl + 1) * NE], in_=lgT[:, tl * 128:(tl + 1) * 128], identity=ident_f[0:NE, 0:NE])
                    pd = PE.mark(ins)
                    DVE.wait(pd)
                    c0 = DVE.mark(DVE.e.tensor_copy(out=lg[:], in_=psb[b][:, 0:8 * NE].rearrange("p (t e) -> p t e", e=NE)))
                    bank_free[b] = c0
                    DVE.wait(c0)

                    def bc(a):
                        return a[:, :].unsqueeze(2).broadcast_to([128, 8, NE])
                    x1 = DVE.mark(DVE.e.tensor_reduce(out=sm["m1"][:], in_=lg[:], axis=AX.X, op=ALU.max)); DVE.wait(x1)
                    x1 = DVE.mark(DVE.e.tensor_tensor(out=big["eq1"][:], in0=lg[:], in1=bc(sm["m1"]), op=ALU.is_equal)); DVE.wait(x1)
                    x1 = DVE.mark(DVE.e.scalar_tensor_tensor(out=big["l2"][:], in0=big["eq1"][:], scalar=-1e30, in1=lg[:], op0=ALU.mult, op1=ALU.add)); DVE.wait(x1)
                    x1 = DVE.mark(DVE.e.tensor_reduce(out=sm["m2"][:], in_=big["l2"][:], axis=AX.X, op=ALU.max)); DVE.wait(x1)
                    x1 = DVE.mark(DVE.e.tensor_tensor(out=big["eq2"][:], in0=big["l2"][:], in1=bc(sm["m2"]), op=ALU.is_equal)); DVE.wait(x1)
                    x1 = DVE.mark(DVE.e.tensor_tensor(out=sm["d"][:], in0=sm["m2"][:], in1=sm["m1"][:], op=ALU.subtract))
                    ACT.wait(x1)
                    x2 = ACT.mark(ACT.e.activation(out=sm["e"][:], in_=sm["d"][:], func=AF.Exp))
                    DVE.wait(x2)
                    x1 = DVE.mark(DVE.e.tensor_scalar(out=sm["w1"][:], in0=sm["e"][:], scalar1=1.0, scalar2=0.0, op0=ALU.add, op1=ALU.add)); DVE.wait(x1)
                    x1 = DVE.mark(DVE.e.reciprocal(out=sm["w1"][:], in_=sm["w1"][:])); DVE.wait(x1)
                    x1 = DVE.mark(DVE.e.tensor_tensor(out=sm["w2"][:], in0=sm["e"][:], in1=sm["w1"][:], op=ALU.mult)); DVE.wait(x1)
                    x1 = DVE.mark(DVE.e.tensor_tensor(out=big["g1"][:], in0=big["eq1"][:], in1=bc(sm["w1"]), op=ALU.mult)); DVE.wait(x1)
                    x1 = DVE.mark(DVE.e.tensor_tensor(out=gates[:], in0=big["eq2"][:], in1=bc(sm["w2"]), op=ALU.mult)); DVE.wait(x1)
                    x1 = DVE.mark(DVE.e.tensor_tensor(out=gates[:], in0=gates[:], in1=big["g1"][:], op=ALU.add))
                    PE.wait(x1)
                    bb2 = [bank_acquire(), bank_acquire()]
                    for tl in range(8):
                        ins = PE.e.transpose(out=psb[bb2[tl // 4]][0:NE, (tl % 4) * 128:(tl % 4 + 1) * 128], in_=gates[:, tl, :], identity=ident_f[:, :])
                    pd = PE.mark(ins)
                    DVE.wait(pd)
                    g0 = DVE.mark(DVE.e.tensor_copy(out=gT[:, 0:512], in_=psb[bb2[0]][0:NE, :]))
                    gtd = DVE.mark(DVE.e.tensor_copy(out=gT[:, 512:1024], in_=psb[bb2[1]][0:NE, :]))
                    bank_free[bb2[0]] = g0
                    bank_free[bb2[1]] = gtd
                    if stop_after == f"route_{l}" and half == 0:
                        POOL.wait(gtd)
                        POOL.dma(dbg_out[0:NE, 0:1024], gT[:], misc)
                        POOL.dma(dbg_out[NE:2 * NE, 0:1024], lgT[:], misc)
                        SP.wait((misc, misc.total))
                        return True
                    for e in range(NE):
                        q = e % 2
                        DVE.wait(gate_free[q], gtd)
                        sd = DVE.mark(DVE.e.tensor_scalar(out=sel8[q][:], in0=ones8[:], scalar1=ident_f[0:NE, e:e + 1], scalar2=0.0, op0=ALU.mult, op1=ALU.add))
                        PE.wait(sd)
                        gds = []
                        for tb in range(2):
                            b = bank_acquire()
                            pd = PE.mark(PE.e.matmul(psb[b][:, :], lhsT=sel8[q][:], rhs=gT[:, tb * 512:(tb + 1) * 512], start=True, stop=True))
                            ACT.wait(pd, gate_free[q])
                            gd = ACT.mark(ACT.e.activation(out=gate_b[q][:, tb * 512:(tb + 1) * 512], in_=psb[b][:, :], func=AF.Copy))
                            bank_free[b] = gd
                            gds.append(gd)
                        DVE.wait(gds)
                        ffn_half(ph, hTh, WT("moe_w_gate")[j, e], WT("moe_w_up")[j, e], WT("moe_w_down")[j, e], MOD(l, 5), tok0, aT, s_t, gate_b=gate_b[q])
                        gate_free[q] = [(DVE.sem, DVE.sem.total), (PE.sem, PE.sem.total)]
                finish_stores()
                barrier()
        return False


    def kv_phase():
        with contextlib.ExitStack() as ph:
            hT = k.sb("hT", [128, KC, S], BF16, ph)
            with contextlib.ExitStack() as ph2:
                phase_norm(ph2, V("kvng"), KVMOD(0), KVMOD(1), hT, 0, S)
                barrier()
            wkvf = WT("w_kvf")
            stg = [k.sb(f"kst{q}", [128, 512], BF16, ph) for q in range(3)]
            stg_ring = Ring(k, "kst", [t[:] for t in stg], es=ph)
            cnt = [0]

            def evac(out, in_, waits):
                E = ACT if cnt[0] % 2 == 0 else DVE
                cnt[0] += 1
                E.wait(*waits)
                if E is ACT:
                    return ACT.mark(ACT.e.activation(out=out, in_=in_, func=AF.Copy))
                return DVE.mark(DVE.e.tensor_copy(out=out, in_=in_))

            def wloadk(t, slot):
                sid = wring_slot_of(slot)
                sv = slot[:, 0:KC * 128].rearrange("p (kc n) -> p kc n", kc=KC)
                d1 = POOL.dma(sv, wkvf[:, t * 128:(t + 1) * 128].rearrange("(kc p) n -> p kc n", p=128), wring.dsems[sid])
                return [d1], [sv]

            def epik(t, tb, banks, rdy):
                s = stg_ring.next()
                c = evac(stg[s][:], psb[banks[0]][:, :], [rdy[0], stg_ring.free[s]])
                SP.wait(c)
                stg_ring.free[s] = SP.dma(KTs[t * 128:(t + 1) * 128, tb * 512:(tb + 1) * 512], stg[s][:], stg_ring.dsems[s])
                return [c]

            run_gemm(range(H), 1, KC, wloadk, lambda kc, tb: hT[:, kc, tb * 512:(tb + 1) * 512], range(4), epik)
            vst = [k.sb(f"vst{q}", [128, 256], BF16, ph) for q in range(3)]
            vring = Ring(k, "vst", [t[:] for t in vst], es=ph)
            for cb in range(8):
                s = wring.next()
                POOL.wait(wring.free[s])
                sv = wring.views[s][:, 0:KC * 256].rearrange("p (kc n) -> p kc n", kc=KC)
                dep = POOL.dma(sv, wkvf[:, D + cb * 256: D + (cb + 1) * 256].rearrange("(kc p) n -> p kc n", p=128), wring.dsems[s])
                PE.wait(dep)
                for tt in range(16):
                    b = bank_acquire()
                    for kc in range(KC):
                        ins = PE.e.matmul(psb[b][:, 0:256], lhsT=hT[:, kc, tt * 128:(tt + 1) * 128], rhs=sv[:, kc, :],
                                          start=(kc == 0), stop=(kc == KC - 1))
                    rdy = PE.mark(ins)
                    q = vring.next()
                    c = evac(vst[q][:], psb[b][:, 0:256], [rdy, vring.free[q]])
                    bank_free[b] = c
                    SP.wait(c)
                    vring.free[q] = SP.dma(Vs[tt * 128:(tt + 1) * 128, cb * 256:(cb + 1) * 256], vst[q][:], vring.dsems[q])
                wring.free[s] = rdy
            wf = k.sb("wf", [128, KC, H], BF16, ph)
            dwf = POOL.dma(wf[:], wkvf[:, 2 * D:2 * D + H].rearrange("(kc p) n -> p kc n", p=128), misc)
            bft = k.sb("bft", [H, 1], F32, ph)
            nbf = k.sb("nbf", [H, 1], F32, ph)
            sp = k.sb("sp", [H, S], F32, ph)
            ones16 = k.sb("ones16", [H, S], F32, ph)
            PT = k.sb("PT", [H, S], F32, ph)
            d = SP.dma(bft[:], bf_d[:, :], misc)
            DVE.wait(d)
            DVE.e.memset(ones16[:], 1.0)
            nb = DVE.mark(DVE.e.tensor_scalar(out=nbf[:], in0=bft[:], scalar1=-1.0, scalar2=0.0, op0=ALU.mult, op1=ALU.add))
            PE.wait(dwf)
            for tb in range(4):
                b = bank_acquire()
                for kc in range(KC):
                    ins = PE.e.matmul(psb[b][0:H, :], lhsT=wf[:, kc, :], rhs=hT[:, kc, tb * 512:(tb + 1) * 512],
                                      start=(kc == 0), stop=(kc == KC - 1))
                rdy = PE.mark(ins)
                ACT.wait(rdy, nb)
                e1 = ACT.mark(ACT.e.activation(out=sp[:, tb * 512:(tb + 1) * 512], in_=psb[b][0:H, :], func=AF.Exp,
                                               bias=nbf[:, 0:1], scale=-1.0))
                bank_free[b] = e1
                ACT.wait(e1)
                e2 = ACT.mark(ACT.e.activation(out=sp[:, tb * 512:(tb + 1) * 512], in_=sp[:, tb * 512:(tb + 1) * 512], func=AF.Ln,
                                               bias=1.0, scale=1.0))
            DVE.wait(e2)
            scn = DVE.mark(DVE.e.tensor_tensor_scan(out=PT[:], data0=ones16[:], data1=sp[:], initial=0.0, op0=ALU.mult, op1=ALU.add))
            DVE.wait(scn)
            DVE.mark(DVE.e.tensor_scalar(out=GqT[:], in0=PT[:], scalar1=-float(np.sqrt(HD)), scalar2=0.0, op0=ALU.mult, op1=ALU.add))
            PE.wait(scn)
            b = bank_acquire()
            for tt in range(16):
                ins = PE.e.transpose(out=psb[b][:, tt * H:(tt + 1) * H], in_=PT[:, tt * 128:(tt + 1) * 128], identity=ident_f[0:H, 0:H])
            rdy = PE.mark(ins)
            DVE.wait(rdy)
            bank_free[b] = DVE.mark(DVE.e.tensor_copy(out=negF[:], in_=psb[b][:, 0:16 * H]))
            for r_ in (stg_ring, vring):
                for q in range(3):
                    SP.wait((r_.dsems[q], r_.dsems[q].total))
            if stop_after == "kv":
                POOL.e.wait_ge(bar.h, bar.total)
                barrier()
                POOL.e.wait_ge(bar.h, bar.total)
                POOL.dma(dbg_out[0:H, :], PT[:], misc)
                POOL.dma(dbg_out[128:256, 0:256], negF[:], misc)
                POOL.dma(dbg_out[256:256 + H, :], GqT[:], misc)
                SP.wait((misc, misc.total))
                return True
            barrier()
        return False

    def attn_layer(l):
        i = l - 2
        with contextlib.ExitStack() as ph:
            hT = k.sb("hT", [128, KC, S], BF16, ph)
            with contextlib.ExitStack() as ph2:
                phase_norm(ph2, V(f"nmix{l}"), MOD(l, 0), MOD(l, 1), hT, 0, S)
                barrier()
            qT = k.sb("qT", [128, H, S], BF16, ph)
            cnt = [0]

            def wloadq(t, slot):
                sid = wring_slot_of(slot)
                sv = slot[:, 0:KC * 128].rearrange("p (kc n) -> p kc n", kc=KC)
                d1 = POOL.dma(sv, WT("attn_wq")[i][:, t * 128:(t + 1) * 128].rearrange("(kc p) n -> p kc n", p=128), wring.dsems[sid])
                return [d1], [sv]

            def epiq(t, tb, banks, rdy):
                E = ACT if cnt[0] % 2 == 0 else DVE
                cnt[0] += 1
                E.wait(rdy[0])
                if E is ACT:
                    c = ACT.mark(ACT.e.activation(out=qT[:, t, tb * 512:(tb + 1) * 512], in_=psb[banks[0]][:, :], func=AF.Copy))
                else:
                    c = DVE.mark(DVE.e.tensor_copy(out=qT[:, t, tb * 512:(tb + 1) * 512], in_=psb[banks[0]][:, :]))
                return [c]

            run_gemm(range(H), 1, KC, wloadq, lambda kc, tb: hT[:, kc, tb * 512:(tb + 1) * 512], range(4), epiq)
            barrier()
            oT = hT
            phs = contextlib.ExitStack()
            KT = [k.sb(f"KT{q}", [128, S], BF16, phs) for q in range(2)]
            Vh = [k.sb(f"Vh{q}", [128, 16, 128], BF16, phs) for q in range(2)]
            kvring = Ring(k, "kv", [None, None], es=phs)
            pT = [k.sb(f"pT{q}", [128, 512], BF16, phs) for q in range(4)]
            pT_free = [None] * 4
            pi = [0]
            selh = [k.sb(f"selh{q}", [H, 128], BF16, phs) for q in range(2)]
            sel_free = [None, None]
            ones16b = k.sb("ones16b", [H, 128], F32, phs)
            rd = [k.sb(f"rd{q}", [128, 512], F32, phs) for q in range(2)]
            rd_free = [None, None]
            ri = [0]
            o1 = DVE.mark(DVE.e.memset(ones16b[:], 1.0))
            DVE.wait(o1)
            scale = float(HD ** -0.5)
            acc_i = [0]; sc_i = [0]
            for h in range(H):
                s = kvring.next()
                SP.wait(kvring.free[s])
                d1 = SP.dma(KT[s][:], KTs[h * 128:(h + 1) * 128, :], kvring.dsems[s])
                d2 = SP.dma(Vh[s][:], Vs[:, h * 128:(h + 1) * 128].rearrange("(i p) d -> p i d", p=128), kvring.dsems[s])
                q2 = h % 2
                DVE.wait(sel_free[q2])
                sd = DVE.mark(DVE.e.tensor_scalar(out=selh[q2][:], in0=ones16b[:], scalar1=ident_f[0:H, h:h + 1], scalar2=0.0,
                                                  op0=ALU.mult, op1=ALU.add))
                PE.wait(d1, d2, sd)
                fin = None
                for j in range(4):
                    acc_i[0] += 1
                    bo, bd = (0, 1) if acc_i[0] % 2 == 0 else (2, 3)
                    PE.wait(bank_free[bo], bank_free[bd])
                    ntile = 4 * (j + 1)

                    def qk(ii):
                        o = max(0, ii - 4 * j)
                        c0 = 128 * o
                        sc_i[0] += 1
                        b = 4 + sc_i[0] % 4
                        PE.wait(bank_free[b])
                        PE.e.matmul(psb[b][:, c0:512], lhsT=KT[s][:, ii * 128:(ii + 1) * 128], rhs=qT[:, h, j * 512 + c0:(j + 1) * 512],
                                    start=True, stop=False)
                        ins = PE.e.matmul(psb[b][:, c0:512], lhsT=selh[q2][:], rhs=GqT[:, j * 512 + c0:(j + 1) * 512], start=False, stop=True)
                        return b, c0, PE.mark(ins)

                    pend = []
                    nxt = 0
                    while nxt < min(3, ntile):
                        pend.append(qk(nxt))
                        nxt += 1
                    for ii in range(ntile):
                        b, c0, rdy = pend.pop(0)
                        if nxt < ntile:
                            pend.append(qk(nxt))
                            nxt += 1
                        p = pi[0] % 4
                        pi[0] += 1
                        ACT.wait(rdy, pT_free[p])
                        e = ACT.mark(ACT.e.activation(out=pT[p][:, c0:512], in_=psb[b][:, c0:512], func=AF.Exp,
                                                      bias=negF[:, ii * H + h: ii * H + h + 1], scale=scale))
                        bank_free[b] = e
                        dep = e
                        if ii >= 4 * j:
                            DVE.wait(e)
                            dep = DVE.mark(DVE.e.tensor_tensor(out=pT[p][:, c0:c0 + 128], in0=pT[p][:, c0:c0 + 128], in1=tri_b[:], op=ALU.mult))
                        PE.wait(dep)
                        PE.e.matmul(psb[bo][:, c0:512], lhsT=Vh[s][:, ii, :], rhs=pT[p][:, c0:512], start=(ii == 0), stop=(ii == ntile - 1))
                        ins = PE.e.matmul(psb[bd][:, c0:512], lhsT=ones_b[:], rhs=pT[p][:, c0:512], start=(ii == 0), stop=(ii == ntile - 1))
                        pT_free[p] = PE.mark(ins)
                    fin = pT_free[p]
                    r = ri[0] % 2
                    ri[0] += 1
                    DVE.wait(fin, rd_free[r])
                    r1 = DVE.mark(DVE.e.reciprocal(out=rd[r][:], in_=psb[bd][:, :]))
                    DVE.wait(r1)
                    r2 = DVE.mark(DVE.e.tensor_tensor(out=oT[:, h, j * 512:(j + 1) * 512], in0=psb[bo][:, :], in1=rd[r][:], op=ALU.mult))
                    rd_free[r] = r2
                    bank_free[bo] = r2
                    bank_free[bd] = r1
                kvring.free[s] = fin
                sel_free[q2] = fin
            barrier()
            phs.close()
            if stop_after == f"att_{l}":
                POOL.e.wait_ge(bar.h, bar.total)
                POOL.dma(dbg_out[:, :].rearrange("(c p) t -> p c t", p=128), oT[:, :, :], misc)
                SP.wait((misc, misc.total))
                return True

            def wloado(t, slot):
                sid = wring_slot_of(slot)
                sv = slot[:, 0:KC * 128].rearrange("p (kc n) -> p kc n", kc=KC)
                d1 = POOL.dma(sv, WT("attn_wo")[i][:, t * 128:(t + 1) * 128].rearrange("(kc p) n -> p kc n", p=128), wring.dsems[sid])
                return [d1], [sv]

            pre, epi = make_res_epi(MOD(l, 2), None, 0)
            run_gemm(range(KC), 1, KC, wloado, lambda kc, tb: oT[:, kc, tb * 512:(tb + 1) * 512], range(4), epi, pre=pre)
            finish_stores()
            barrier()
        return False

    def final_phase(outT):
        with contextlib.ExitStack() as ph:
            zer = k.sb("zer", [128, KC], F32, ph)
            z0 = DVE.mark(DVE.e.memset(zer[:], 0.0))
            DVE.wait(z0)
            last = phase_norm(ph, V("fng"), None, zer[:], None, 0, S, out_dram=outT)
            SP.wait(last)
            for q in range(3):
                pass
        return last

    def run_seq():
        outT = outT_all[cur[0]]
        seq_setup()
        phase_mod()
        barrier()
        if stop_after == "mod":
            SP.dma(dbg_out[:, 0:416], modall[:, :], misc)
            SP.wait((misc, misc.total))
            return True
        done = False
        if stop_after == "only_attn":
            assert not kv_phase()
            assert not attn_layer(2)
            finish_stores()
            barrier()
            d = SP.dma(outT, xTs[:, :], misc)
            SP.wait(d)
            return True
        for l in range(4):
            if l == 2:
                if kv_phase():
                    return True
            r_ = conv_layer(l) if l < 2 else attn_layer(l)
            if r_:
                return True
            if stop_after == f"mix_{l}":
                done = True
                break
            if ffn_layer(l):
                return True
            if stop_after == f"ffn_{l}":
                done = True
                break
        finish_stores()
        barrier()
        if done:
            d = SP.dma(outT, xTs[:, :], misc)
            SP.wait(d)
            return True
        final_phase(outT)
        barrier()
        return False

    for si in range(nseq):
        cur[0] = si
        if run_seq():
            return


def _prep_common(inp):
    g = lambda n: np.asarray(inp[n], np.float32)
    dww = np.zeros((2, 128, CW * 16), np.float32)
    for i in range(2):
        for kk in range(CW):
            dww[i][:, kk * 16:(kk + 1) * 16] = _fm(g("conv_dw_w")[i, kk])
    rw = np.zeros((2, 128, KC * NE), np.float32)
    for j in range(2):
        rw[j] = g("moe_router_w")[j].reshape(KC, 128, NE).transpose(1, 0, 2).reshape(128, KC * NE)
    rb = g("moe_router_b").reshape(2, NE, 1).copy()
    common = dict(
        dww=dww, bf=g("b_f").reshape(H, 1).copy(), rw=rw, rb=rb,
        ident=np.eye(128, dtype=np.float32), tri=np.triu(np.ones((128, 128), np.float32)),
        ada_w=g("ada_w"), conv_pw1_w=g("conv_pw1_w"), conv_pw2_w=g("conv_pw2_w"), kv_ada_w=g("kv_ada_w"),
        w_kvf=g("w_kvf"), attn_wq=g("attn_wq"), attn_wo=g("attn_wo"), ffn_w_gate=g("ffn_w_gate"),
        ffn_w_up=g("ffn_w_up"), ffn_w_down=g("ffn_w_down"), moe_w_gate=g("moe_w_gate"),
        moe_w_up=g("moe_w_up"), moe_w_down=g("moe_w_down"),
    )
    return common


def _vecs_for(inp, b):
    g = lambda n: np.asarray(inp[n], np.float32)
    v = np.zeros((128, NV * 16), np.float32)

    def put(name, vec):
        i = VIDX[name]
        v[:, i * 16:(i + 1) * 16] = _fm(vec)
    put("c", g("c")[b])
    for l in range(4):
        for s in range(6):
            put(f"ab{l}_{s}", g("ada_b")[l, s * D:(s + 1) * D])
        put(f"nmix{l}", g("norm_mix_g")[l]); put(f"nffn{l}", g("norm_ffn_g")[l])
    put("kvab_0", g("kv_ada_b")[:D]); put("kvab_1", g("kv_ada_b")[D:])
    for i in range(2):
        put(f"pw1bv{i}", g("conv_pw1_b")[i, :D]); put(f"pw1bg{i}", g("conv_pw1_b")[i, D:])
        put(f"dwb{i}", g("conv_dw_b")[i]); put(f"lng{i}", g("conv_ln_g")[i]); put(f"lnb{i}", g("conv_ln_b")[i])
        put(f"pw2b{i}", g("conv_pw2_b")[i])
    put("kvng", g("kv_norm_g")); put("fng", g("final_norm_g"))
    return v


def make_in_maps(inp, seqs_per_core, names=None):
    common = _prep_common(inp)
    if names is not None:
        common = {k_: v_ for k_, v_ in common.items() if k_ in names}
    x = np.asarray(inp["x"], np.float32)
    maps = []
    for seqs in seqs_per_core:
        m = dict(common)
        m["xT"] = np.stack([np.ascontiguousarray(x[b].T) for b in seqs], 0)
        m["vecs"] = np.stack([_vecs_for(inp, b) for b in seqs], 0)
        maps.append(m)
    return maps


NUSED = 8
NSEQ = 8 // NUSED


def kernel(**inputs):
    nc = build(nseq=NSEQ)
    seqs = [list(range(c * NSEQ, (c + 1) * NSEQ)) for c in range(NUSED)]
    in_maps = make_in_maps(inputs, seqs)
    res = run_bass_kernel_spmd(nc, in_maps, core_ids=list(range(NUSED)))
    out = np.zeros((8, S, D), np.float32)
    for c in range(NUSED):
        o = np.asarray(res.results[c]["outT"])
        for i, b in enumerate(seqs[c]):
            out[b] = o[i].T
    return out
```

```python
import contextlib
import numpy as np
import concourse.bass as bass
import concourse.mybir as mybir
from concourse.bass_utils import run_bass_kernel_spmd

F32, BF16 = mybir.dt.float32, mybir.dt.bfloat16
AF = mybir.ActivationFunctionType
ALU = mybir.AluOpType
AX = mybir.AxisListType

D = 2048; S = 2048; H = 16; HD = 128; CW = 31; DFF = 5632; NE = 8
KC = D // 128; FC = DFF // 128
EPS = 1e-6
NCORES = 8
WSLOT = FC * 128

VEC_NAMES = ["c"]
for _l in range(4):
    VEC_NAMES += [f"ab{_l}_{s}" for s in range(6)]
VEC_NAMES += ["kvab_0", "kvab_1"]
for _l in range(4):
    VEC_NAMES += [f"nmix{_l}", f"nffn{_l}"]
for _i in range(2):
    VEC_NAMES += [f"pw1bv{_i}", f"pw1bg{_i}", f"dwb{_i}", f"lng{_i}", f"lnb{_i}", f"pw2b{_i}"]
VEC_NAMES += ["kvng", "fng"]
VIDX = {n: i for i, n in enumerate(VEC_NAMES)}
NV = len(VEC_NAMES)


def _fm(v):
    return np.ascontiguousarray(np.asarray(v, np.float32).reshape(KC, 128).T)


class Sem:
    def __init__(self, h):
        self.h = h
        self.total = 0


class Eng:
    def __init__(self, k, e, name):
        self.k = k; self.e = e; self.name = name
        self.sem = k.newsem("p_" + name)
        self.seen = {}

    def wait(self, *deps):
        for d in deps:
            if d is None:
                continue
            if isinstance(d, list):
                self.wait(*d)
                continue
            sem, v = d
            if self.seen.get(id(sem), 0) >= v:
                continue
            self.e.wait_ge(sem.h, v)
            self.seen[id(sem)] = v

    def mark(self, ins):
        ins.then_inc(self.sem.h, 1)
        self.sem.total += 1
        return (self.sem, self.sem.total)

    def dma(self, out, in_, dsem):
        ins = self.e.dma_start(out=out, in_=in_)
        ins.then_inc(dsem.h, 16)
        dsem.total += 16
        return (dsem, dsem.total)


class Ring:
    def __init__(self, k, name, views, dma=True, es=None):
        self.views = views; self.n = len(views)
        self.dsems = [k.newsem(f"{name}{i}", es) for i in range(self.n)] if dma else None
        self.free = [None] * self.n
        self.i = 0

    def next(self):
        s = self.i % self.n
        self.i += 1
        return s


class K:
    def __init__(self):
        self.nc = bass.Bass("TRN2", target_bir_lowering=False)
        self.es = contextlib.ExitStack()
        self.nsem = 0

    def newsem(self, name, es=None):
        pool = self.__dict__.setdefault("sempool", [])
        if es is not None and pool:
            sem = pool.pop()
        else:
            self.nsem += 1
            sem = Sem(self.es.enter_context(self.nc.semaphore(f"{name}_{self.nsem}")))
        if es is not None:
            es.callback(pool.append, sem)
        return sem

    def sb(self, name, shape, dt, es=None):
        self.nsb = getattr(self, "nsb", 0) + 1
        return (es or self.es).enter_context(self.nc.sbuf_tensor(f"{name}_s{self.nsb}", list(shape), dt))


def build(stop_after=None, dbg=None, nseq=1):
    k = K()
    nc = k.nc
    with k.es:
        _build(k, nc, stop_after, dbg, nseq)
    return nc


def _build(k, nc, stop_after, dbg, nseq):
    def din(name, shape, dt=F32):
        return nc.dram_tensor(name, list(shape), dt, kind="ExternalInput").ap()

    xT_in = din("xT", [nseq, D, S])
    vecs_d = din("vecs", [nseq, 128, NV * 16])
    cur = [0]
    dww_d = din("dww", [2, 128, CW * 16])
    bf_d = din("bf", [16, 1])
    rw_d = din("rw", [2, 128, KC * NE])
    rb_d = din("rb", [2, NE, 1])
    ident_d = din("ident", [128, 128])
    tri_d = din("tri", [128, 128])
    WSHAPES = dict(ada_w=[4, D, 6 * D], conv_pw1_w=[2, D, 2 * D], conv_pw2_w=[2, D, D], kv_ada_w=[D, 2 * D],
                   w_kvf=[D, 2 * D + H], attn_wq=[2, D, D], attn_wo=[2, D, D], ffn_w_gate=[2, D, DFF],
                   ffn_w_up=[2, D, DFF], ffn_w_down=[2, DFF, D], moe_w_gate=[2, NE, D, DFF],
                   moe_w_up=[2, NE, D, DFF], moe_w_down=[2, NE, DFF, D])
    wcache = {}

    def WT(name):
        if name not in wcache:
            wcache[name] = din(name, WSHAPES[name])
        return wcache[name]
    outT_all = nc.dram_tensor("outT", [nseq, D, S], F32, kind="ExternalOutput").ap()
    dbg_out = None
    if dbg is not None:
        dbg_out = nc.dram_tensor("dbg", list(dbg), F32, kind="ExternalOutput").ap()
    xTs = nc.dram_tensor("xTs", [D, S], F32, kind="Internal").ap()
    KTs = nc.dram_tensor("KTs", [D, S], BF16, kind="Internal").ap()
    Vs = nc.dram_tensor("Vs", [S, D], BF16, kind="Internal").ap()

    PE = Eng(k, nc.tensor, "pe"); ACT = Eng(k, nc.scalar, "act"); DVE = Eng(k, nc.vector, "dve")
    POOL = Eng(k, nc.gpsimd, "pool"); SP = Eng(k, nc.sync, "sp")
    bar = k.newsem("bar")
    BAR_ENGS = [PE, ACT, DVE, SP]

    def barrier():
        for e in BAR_ENGS:
            e.e.drain().then_inc(bar.h, 1)
        bar.total += len(BAR_ENGS)
        for e in BAR_ENGS:
            e.e.wait_ge(bar.h, bar.total)

    vecs = k.sb("vecs", [128, NV * 16], F32)
    modall = k.sb("modall", [128, 4 * 96 + 32], F32)
    ident_f = k.sb("ident_f", [128, 128], F32)
    ident_b = k.sb("ident_b", [128, 128], BF16)
    tri_b = k.sb("tri_b", [128, 128], BF16)
    ones_b = k.sb("ones_b", [128, 128], BF16)
    cact = k.sb("cact", [128, KC], BF16)
    eps_c = k.sb("eps_c", [128, 1], F32)
    deps_c = k.sb("deps_c", [128, 1], F32)
    negF = k.sb("negF", [128, 16 * H], F32)
    GqT = k.sb("GqT", [H, S], BF16)
    wring_t = k.sb("wring", [128, 3 * WSLOT], BF16)
    wring = Ring(k, "w", [wring_t[:, i * WSLOT:(i + 1) * WSLOT] for i in range(3)])
    psb = [k.es.enter_context(nc.psum_tensor(f"ps{i}", [128, 512], F32)) for i in range(8)]
    bank_free = [None] * 8
    bank_i = [0]
    misc = k.newsem("misc")

    def V(name):
        i = VIDX[name]
        return vecs[:, i * 16:(i + 1) * 16]

    def MOD(l, s):
        return modall[:, l * 96 + s * 16: l * 96 + (s + 1) * 16]

    def KVMOD(s):
        return modall[:, 384 + s * 16: 384 + (s + 1) * 16]

    nbanks = [8]

    def bank_acquire():
        b = bank_i[0] % nbanks[0]
        bank_i[0] += 1
        PE.wait(bank_free[b])
        return b

    def selfwait(E, dep):
        E.wait(dep)

    d0 = SP.dma(ident_f[:], ident_d[:, :], misc)
    tri_f = k.sb("tri_f", [128, 128], F32)
    d0 = SP.dma(tri_f[:], tri_d[:, :], misc)
    DVE.wait(d0)
    DVE.e.tensor_copy(out=ident_b[:], in_=ident_f[:])
    DVE.e.tensor_copy(out=tri_b[:], in_=tri_f[:])
    DVE.e.memset(eps_c[:], float(EPS))
    DVE.e.memset(deps_c[:], float(D * EPS))
    m = DVE.mark(DVE.e.memset(ones_b[:], 1.0))
    PE.wait(m); ACT.wait(m); SP.wait(m)
    barrier()

    def seq_setup():
        dv = SP.dma(vecs[:], vecs_d[cur[0]], misc)
        dx = SP.dma(xTs[:, :], xT_in[cur[0]], misc)
        ACT.wait(dv)
        cdep = ACT.mark(ACT.e.activation(out=cact[:], in_=V("c"), func=AF.Silu))
        SP.wait(dx)
        PE.wait(cdep); DVE.wait(dv)
        barrier()

    mod_groups = []
    mod_state = dict(pos=0, acc=0.0, rate=0.0, tiles=[], limit=0)

    def mod_init():
        mod_groups.clear()
        mod_groups.extend([(WT("ada_w")[l], 96, l * 96, VIDX[f"ab{l}_0"]) for l in range(4)])
        mod_groups.insert(2, (WT("kv_ada_w"), 32, 384, VIDX["kvab_0"]))
        mod_state["tiles"] = [(gi, j2) for gi, g in enumerate(mod_groups) for j2 in range(g[1] // 2)]
        mod_state["pos"] = 0

    def mod_emit_tile(b):
        gi, j2 = mod_state["tiles"][mod_state["pos"]]
        mod_state["pos"] += 1
        (w, nch, off, vi) = mod_groups[gi]
        if j2 == 0:
            PE.wait(bank_free[b])
        s = wring.next()
        POOL.wait(wring.free[s])
        sv = wring.views[s][:, 0:KC * 256].rearrange("p (kc n) -> p kc n", kc=KC)
        dep = POOL.dma(sv, w[:, j2 * 256:(j2 + 1) * 256].rearrange("(kc p) n -> p kc n", p=128), wring.dsems[s])
        PE.wait(dep)
        for jj in range(2):
            j = j2 * 2 + jj
            for kc in range(KC):
                ins = PE.e.matmul(psb[b][:, j:j + 1], lhsT=sv[:, kc, jj * 128:(jj + 1) * 128], rhs=cact[:, kc:kc + 1],
                                  start=(kc == 0), stop=(kc == KC - 1))
        wring.free[s] = PE.mark(ins)
        if j2 == nch // 2 - 1:
            DVE.wait(wring.free[s])
            bank_free[b] = DVE.mark(DVE.e.tensor_tensor(out=modall[:, off:off + nch], in0=psb[b][:, 0:nch],
                                                        in1=vecs[:, vi * 16: vi * 16 + nch], op=ALU.add))

    def mod_tiles_upto(ngroups):
        return sum(g[1] // 2 for g in mod_groups[:ngroups])

    def phase_mod():
        mod_init()
        b = bank_acquire()
        while mod_state["pos"] < mod_tiles_upto(1):
            mod_emit_tile(b)

    def mod_side():
        mod_state["acc"] += mod_state["rate"]
        while mod_state["acc"] >= 1.0:
            mod_state["acc"] -= 1.0
            if mod_state["pos"] < mod_state["limit"]:
                mod_emit_tile(7)

    def mod_flush(ngroups):
        while mod_state["pos"] < mod_tiles_upto(ngroups):
            mod_emit_tile(7)

    def phase_norm(ph, g_ap, sh_ap, sc_ap, hT, tok0, ntok, router=None, out_dram=None, scratch=None):
        off = [0]

        def alloc(name, shape, dt):
            if scratch is None:
                return k.sb(name, shape, dt, ph)
            n = int(np.prod(shape[1:])) * (2 if dt == F32 else 1)
            v = scratch[:, off[0]:off[0] + n]
            off[0] += n
            if dt == F32:
                v = v.bitcast(F32)
            if len(shape) == 3:
                v = v.rearrange("p (a b) -> p a b", a=shape[1])
            return v
        avec = k.sb("avec", [128, KC], F32, ph)
        xt = [alloc(f"xt{i}", [128, KC, 512], F32) for i in range(2)]
        xring = Ring(k, "nx", [t[:] for t in xt], es=ph)
        sq = [alloc(f"sq{i}", [128, 4, 512], BF16) for i in range(2)]
        sq_free = [None, None]
        rstd = [alloc(f"rstd{i}", [128, 512], F32) for i in range(2)]
        tmp = [alloc(f"ntmp{i}", [128, 512], F32) for i in range(3)]
        tmp_free = [None] * 3
        rstd_free = [None, None]
        fin_sems = [k.newsem(f"fin{q}", ph) for q in range(3)] if out_dram is not None else None
        a0 = DVE.mark(DVE.e.scalar_tensor_tensor(out=avec[:], in0=sc_ap, scalar=1.0, in1=g_ap, op0=ALU.add, op1=ALU.mult))
        DVE.wait(a0)
        adep = DVE.mark(DVE.e.tensor_scalar(out=avec[:], in0=avec[:], scalar1=float(np.sqrt(D)), scalar2=0.0, op0=ALU.mult, op1=ALU.add))
        DVE.wait(adep)
        nt = ntok // 512
        ti = 0
        last = None
        for tb in range(nt):
            s = xring.next()
            SP.wait(xring.free[s])
            xdep = SP.dma(xring.views[s], xTs[:, tok0 + tb * 512: tok0 + (tb + 1) * 512].rearrange("(c p) t -> p c t", p=128),
                          xring.dsems[s])
            x = xt[s]
            b = bank_acquire()
            ACT.wait(xdep)
            for g4 in range(4):
                q = (tb * 4 + g4) % 2
                ACT.wait(sq_free[q])
                sdep = ACT.mark(ACT.e.activation(out=sq[q][:], in_=x[:, g4 * 4:(g4 + 1) * 4, :], func=AF.Square))
                PE.wait(sdep)
                for c4 in range(4):
                    c = g4 * 4 + c4
                    ins = PE.e.matmul(psb[b][:, :], lhsT=ones_b[:], rhs=sq[q][:, c4, :], start=(c == 0), stop=(c == KC - 1))
                sq_free[q] = PE.mark(ins)
            r = rstd[tb % 2]
            ACT.wait(sq_free[q], rstd_free[tb % 2])
            r0 = ACT.mark(ACT.e.activation(out=r[:], in_=psb[b][:, :], func=AF.Sqrt, bias=deps_c[:, 0:1], scale=1.0))
            bank_free[b] = r0
            DVE.wait(r0)
            rdep = DVE.mark(DVE.e.reciprocal(out=r[:], in_=r[:]))
            DVE.wait(rdep, xdep)
            rb_bank = None
            if router is not None:
                rb_bank = bank_acquire()
            for c in range(KC):
                t = tmp[ti % 3]
                DVE.wait(tmp_free[ti % 3])
                tdep = DVE.mark(DVE.e.scalar_tensor_tensor(out=t[:], in0=x[:, c, :], scalar=avec[:, c:c + 1], in1=r[:],
                                                           op0=ALU.mult, op1=ALU.mult))
                if out_dram is not None:
                    SP.wait(tdep)
                    fdep = SP.dma(out_dram[c * 128:(c + 1) * 128, tok0 + tb * 512: tok0 + (tb + 1) * 512], t[:], fin_sems[ti % 3])
                    tmp_free[ti % 3] = fdep
                    ti += 1
                    last = fdep
                    continue
                ACT.wait(tdep)
                fdep = ACT.mark(ACT.e.activation(out=hT[:, c, tb * 512:(tb + 1) * 512], in_=t[:], func=AF.Identity,
                                                 bias=sh_ap[:, c:c + 1], scale=1.0))
                if router is not None:
                    PE.wait(tdep)
                    ins = PE.e.matmul(psb[rb_bank][0:NE, :], lhsT=router["rw"][:, c, :], rhs=t[:], start=(c == 0), stop=(c == KC - 1))
                    pdep = PE.mark(ins)
                    tmp_free[ti % 3] = [fdep, pdep]
                else:
                    tmp_free[ti % 3] = fdep
                ti += 1
                last = fdep
            xring.free[s] = [tdep]
            rstd_free[tb % 2] = tdep
            if router is not None:
                ACT.wait(pdep)
                bank_free[rb_bank] = ACT.mark(ACT.e.activation(out=router["lgT"][:, tb * 512:(tb + 1) * 512], in_=psb[rb_bank][0:NE, :],
                                                               func=AF.Identity, bias=router["bias"][:, 0:1], scale=1.0))
        return last

    def run_gemm(steps, n_sub, kc_n, wload, rhs, tbs, epi, pre=None, ahead=2, side=None):
        steps = list(steps); tbs = list(tbs)
        its = [(t_, tb_) for t_ in steps for tb_ in tbs]
        if pre is not None:
            for i_ in range(min(ahead, len(its))):
                pre(*its[i_])
        idx = 0
        for t in steps:
            s = wring.next()
            POOL.wait(wring.free[s])
            deps, subs = wload(t, wring.views[s])
            PE.wait(deps)
            for tb in tbs:
                if pre is not None and idx + ahead < len(its):
                    pre(*its[idx + ahead])
                idx += 1
                banks = [bank_acquire() for _ in range(n_sub)]
                rdy = []
                for si in range(n_sub):
                    for kc in range(kc_n):
                        ins = PE.e.matmul(psb[banks[si]][:, :], lhsT=subs[si][:, kc, :], rhs=rhs(kc, tb),
                                          start=(kc == 0), stop=(kc == kc_n - 1))
                    rdy.append(PE.mark(ins))
                fr = epi(t, tb, banks, rdy)
                for bi, b in enumerate(banks):
                    bank_free[b] = fr[bi]
            wring.free[s] = rdy[-1]
            if side is not None:
                side()

    def wload_cols(wmat, ncols_list, kc_n):
        def f(t, slot):
            deps = []; subs = []
            cols = ncols_list(t)
            for i, c0 in enumerate(cols):
                sv = slot[:, i * kc_n * 128:(i + 1) * kc_n * 128].rearrange("p (kc n) -> p kc n", kc=kc_n)
                deps.append(POOL.dma(sv, wmat[:, c0:c0 + 128].rearrange("(kc p) n -> p kc n", p=128), wring.dsems[wring_slot_of(slot)]))
                subs.append(sv)
            return deps, subs
        return f

    slot_ids = {}
    for i in range(3):
        slot_ids[i] = wring.views[i]

    def wring_slot_of(slot):
        for i in range(3):
            if slot is wring.views[i]:
                return i
        raise KeyError

    xs_t = [k.sb(f"xs{i}", [128, 512], F32) for i in range(3)]
    xsring = Ring(k, "xs", [t[:] for t in xs_t])
    xs_store = [k.newsem(f"xst{i}") for i in range(3)]
    et_t = [k.sb(f"et{i}", [128, 512], F32) for i in range(2)]
    et_free = [None, None]
    et_i = [0]

    def make_res_epi(gvec, gbvec, tokbase, gate_b=None, out_dram=None):
        state = {}
        dst = xTs if out_dram is None else out_dram

        def pre(t, tb):
            s = xsring.next()
            SP.wait(xsring.free[s])
            state[(t, tb)] = (s, SP.dma(xsring.views[s], xTs[t * 128:(t + 1) * 128, tokbase + tb * 512: tokbase + (tb + 1) * 512],
                                        xsring.dsems[s]))

        def epi(t, tb, banks, rdy):
            s, xdep = state.pop((t, tb))
            b = banks[0]
            e = et_i[0] % 2
            et_i[0] += 1
            et = et_t[e]
            if gate_b is None:
                ACT.wait(rdy[0], et_free[e])
                if gbvec is None:
                    adep = ACT.mark(ACT.e.activation(out=et[:], in_=psb[b][:, :], func=AF.Identity, scale=gvec[:, t:t + 1]))
                else:
                    adep = ACT.mark(ACT.e.activation(out=et[:], in_=psb[b][:, :], func=AF.Identity, scale=gvec[:, t:t + 1],
                                                     bias=gbvec[:, t:t + 1]))
                DVE.wait(adep, xdep)
                ddep = DVE.mark(DVE.e.tensor_tensor(out=xs_t[s][:], in0=xs_t[s][:], in1=et[:], op=ALU.add))
                et_free[e] = ddep
                fr = adep
            else:
                DVE.wait(rdy[0], et_free[e], xdep)
                a1 = DVE.mark(DVE.e.tensor_tensor(out=et[:], in0=psb[b][:, :], in1=gate_b[:, tb * 512:(tb + 1) * 512], op=ALU.mult))
                DVE.wait(a1)
                ddep = DVE.mark(DVE.e.scalar_tensor_tensor(out=xs_t[s][:], in0=et[:], scalar=gvec[:, t:t + 1], in1=xs_t[s][:],
                                                           op0=ALU.mult, op1=ALU.add))
                et_free[e] = ddep
                fr = a1
            SP.wait(ddep)
            sdep = SP.dma(dst[t * 128:(t + 1) * 128, tokbase + tb * 512: tokbase + (tb + 1) * 512], xs_t[s][:], xs_store[s])
            xsring.free[s] = sdep
            return [fr]
        return pre, epi

    def finish_stores():
        for s in range(3):
            SP.wait((xs_store[s], xs_store[s].total))

    def ffn_half(ph, hT, wg, wu, wd, gvec, tokbase, aT, s_t, gate_b=None):
        s_free = [None, None]
        si = [0]

        def wload_gu(t, slot):
            sid = wring_slot_of(slot)
            svg = slot[:, 0:KC * 128].rearrange("p (kc n) -> p kc n", kc=KC)
            svu = slot[:, KC * 128:2 * KC * 128].rearrange("p (kc n) -> p kc n", kc=KC)
            d1 = POOL.dma(svg, wg[:, t * 128:(t + 1) * 128].rearrange("(kc p) n -> p kc n", p=128), wring.dsems[sid])
            d2 = POOL.dma(svu, wu[:, t * 128:(t + 1) * 128].rearrange("(kc p) n -> p kc n", p=128), wring.dsems[sid])
            return [d1, d2], [svg, svu]

        last_a = [None]

        def epi_gu(t, tb, banks, rdy):
            q = si[0] % 2
            si[0] += 1
            ACT.wait(rdy[0], s_free[q])
            sdep = ACT.mark(ACT.e.activation(out=s_t[q][:], in_=psb[banks[0]][:, :], func=AF.Silu))
            DVE.wait(sdep, rdy[1], ffn_state.get("aT_free"))
            adep = DVE.mark(DVE.e.tensor_tensor(out=aT[:, t, tb * 512:(tb + 1) * 512], in0=psb[banks[1]][:, :], in1=s_t[q][:], op=ALU.mult))
            s_free[q] = adep
            last_a[0] = adep
            return [sdep, adep]

        run_gemm(range(FC), 2, KC, wload_gu, lambda kc, tb: hT[:, kc, tb * 512:(tb + 1) * 512], range(2), epi_gu, side=mod_side)
        PE.wait(last_a[0])

        def wload_d(t, slot):
            sid = wring_slot_of(slot)
            sv = slot[:, 0:FC * 128].rearrange("p (kc n) -> p kc n", kc=FC)
            d1 = POOL.dma(sv, wd[:, t * 128:(t + 1) * 128].rearrange("(kc p) n -> p kc n", p=128), wring.dsems[sid])
            return [d1], [sv]

        pre, epi = make_res_epi(gvec, None, tokbase, gate_b=gate_b)
        run_gemm(range(KC), 1, FC, wload_d, lambda kc, tb: aT[:, kc, tb * 512:(tb + 1) * 512], range(2), epi, pre=pre, side=mod_side)
        ffn_state["aT_free"] = (PE.sem, PE.sem.total)

    ffn_state = {}

    def conv_layer(l):
        i = l
        with contextlib.ExitStack() as ph:
            hT = k.sb("hT", [128, KC, S], BF16, ph)
            with contextlib.ExitStack() as ph2:
                phase_norm(ph2, V(f"nmix{l}"), MOD(l, 0), MOD(l, 1), hT, 0, S)
                barrier()
            if stop_after == f"norm_{l}":
                POOL.e.wait_ge(bar.h, bar.total)
                POOL.dma(dbg_out[:, :].rearrange("(c p) t -> p c t", p=128), hT[:, :, :], misc)
                SP.wait((misc, misc.total))
                return True
            uT = k.sb("uT", [128, KC, S + 32], BF16, ph)
            phs = contextlib.ExitStack()
            DVE.e.memset(uT[:, :, 0:30], 0.0)
            sg_t = [k.sb(f"sg{q}", [128, 512], BF16, phs) for q in range(2)]
            sg_free = [None, None]
            sgi = [0]

            def wload1(t, slot):
                sid = wring_slot_of(slot)
                svv = slot[:, 0:KC * 128].rearrange("p (kc n) -> p kc n", kc=KC)
                svg = slot[:, KC * 128:2 * KC * 128].rearrange("p (kc n) -> p kc n", kc=KC)
                d1 = POOL.dma(svv, WT("conv_pw1_w")[i][:, t * 128:(t + 1) * 128].rearrange("(kc p) n -> p kc n", p=128), wring.dsems[sid])
                d2 = POOL.dma(svg, WT("conv_pw1_w")[i][:, D + t * 128: D + (t + 1) * 128].rearrange("(kc p) n -> p kc n", p=128), wring.dsems[sid])
                return [d1, d2], [svv, svg]

            bv = V(f"pw1bv{i}"); bg = V(f"pw1bg{i}")

            def epi1(t, tb, banks, rdy):
                q = sgi[0] % 2
                sgi[0] += 1
                ACT.wait(rdy[1], sg_free[q])
                sdep = ACT.mark(ACT.e.activation(out=sg_t[q][:], in_=psb[banks[1]][:, :], func=AF.Sigmoid, bias=bg[:, t:t + 1], scale=1.0))
                DVE.wait(sdep, rdy[0])
                udep = DVE.mark(DVE.e.scalar_tensor_tensor(out=uT[:, t, 30 + tb * 512: 30 + (tb + 1) * 512], in0=psb[banks[0]][:, :],
                                                           scalar=bv[:, t:t + 1], in1=sg_t[q][:], op0=ALU.add, op1=ALU.mult))
                sg_free[q] = udep
                return [udep, sdep]

            run_gemm(range(KC), 2, KC, wload1, lambda kc, tb: hT[:, kc, tb * 512:(tb + 1) * 512], range(4), epi1)
            barrier()
            if stop_after == f"pw1_{l}":
                POOL.e.wait_ge(bar.h, bar.total)
                POOL.dma(dbg_out[:, :].rearrange("(c p) t -> p c t", p=128), uT[:, :, 30:30 + S], misc)
                SP.wait((misc, misc.total))
                phs.close()
                return True
            phs.close(); phs = contextlib.ExitStack()
            vT = hT
            dww = k.sb("dww", [128, CW * 16], F32, phs)
            dwd = SP.dma(dww[:], dww_d[i], misc)
            diag = [k.sb(f"diag{q}", [128, CW, 128], BF16, phs) for q in range(2)]
            diag_free = [None, None]
            dwb = V(f"dwb{i}")
            DVE.wait(dwd)
            for j in range(KC):
                q = j % 2
                DVE.wait(diag_free[q])
                for kk in range(CW):
                    ins = DVE.e.tensor_scalar(out=diag[q][:, kk, :], in0=ident_b[:], scalar1=dww[:, kk * 16 + j: kk * 16 + j + 1],
                                              scalar2=0.0, op0=ALU.mult, op1=ALU.add)
                ddep = DVE.mark(ins)
                PE.wait(ddep)
                for tb in range(4):
                    b = bank_acquire()
                    for kk in range(CW):
                        ins = PE.e.matmul(psb[b][:, :], lhsT=diag[q][:, kk, :], rhs=uT[:, j, tb * 512 + kk: tb * 512 + kk + 512],
                                          start=(kk == 0), stop=(kk == CW - 1))
                    rdy = PE.mark(ins)
                    ACT.wait(rdy)
                    bank_free[b] = ACT.mark(ACT.e.activation(out=vT[:, j, tb * 512:(tb + 1) * 512], in_=psb[b][:, :], func=AF.Identity,
                                                             bias=dwb[:, j:j + 1], scale=1.0))
                diag_free[q] = rdy
            barrier()
            if stop_after == f"dw_{l}":
                POOL.e.wait_ge(bar.h, bar.total)
                POOL.dma(dbg_out[:, :].rearrange("(c p) t -> p c t", p=128), vT[:, :, :], misc)
                SP.wait((misc, misc.total))
                phs.close()
                return True
            phs.close(); phs = contextlib.ExitStack()
            sq = [k.sb(f"csq{q}", [128, 4, 512], BF16, phs) for q in range(2)]
            sq_free = [None, None]
            st = {n: k.sb(f"ln_{n}", [128, 512], F32, phs) for n in ["m", "msq", "var", "rstd"]}
            tmp = [k.sb(f"ctmp{q}", [128, 512], F32, phs) for q in range(3)]
            tmp_free = [None] * 3
            lng = V(f"lng{i}"); lnb = V(f"lnb{i}")
            ti = 0
            prev_tb_done = None
            for tb in range(4):
                b1 = bank_acquire(); b2 = bank_acquire()
                for g4 in range(4):
                    q = (tb * 4 + g4) % 2
                    ACT.wait(sq_free[q])
                    sdep = ACT.mark(ACT.e.activation(out=sq[q][:], in_=vT[:, g4 * 4:(g4 + 1) * 4, tb * 512:(tb + 1) * 512], func=AF.Square))
                    PE.wait(sdep)
                    for c4 in range(4):
                        c = g4 * 4 + c4
                        PE.e.matmul(psb[b1][:, :], lhsT=ones_b[:], rhs=vT[:, c, tb * 512:(tb + 1) * 512], start=(c == 0), stop=(c == KC - 1))
                        ins = PE.e.matmul(psb[b2][:, :], lhsT=ones_b[:], rhs=sq[q][:, c4, :], start=(c == 0), stop=(c == KC - 1))
                    sq_free[q] = PE.mark(ins)
                DVE.wait(sq_free[q], prev_tb_done)
                d1 = DVE.mark(DVE.e.tensor_scalar(out=st["m"][:], in0=psb[b1][:, :], scalar1=1.0 / D, scalar2=0.0, op0=ALU.mult, op1=ALU.add))
                DVE.wait(d1)
                d2 = DVE.mark(DVE.e.tensor_tensor(out=st["msq"][:], in0=st["m"][:], in1=st["m"][:], op=ALU.mult))
                DVE.wait(d2)
                d3 = DVE.mark(DVE.e.scalar_tensor_tensor(out=st["var"][:], in0=psb[b2][:, :], scalar=1.0 / D, in1=st["msq"][:],
                                                         op0=ALU.mult, op1=ALU.subtract))
                DVE.wait(d3)
                ACT.wait(d3)
                d4a = ACT.mark(ACT.e.activation(out=st["rstd"][:], in_=st["var"][:], func=AF.Sqrt, bias=eps_c[:, 0:1], scale=1.0))
                DVE.wait(d4a)
                d4 = DVE.mark(DVE.e.reciprocal(out=st["rstd"][:], in_=st["rstd"][:]))
                bank_free[b1] = d4; bank_free[b2] = d4
                DVE.wait(d4)
                for c in range(KC):
                    t = tmp[ti % 3]
                    DVE.wait(tmp_free[ti % 3])
                    e1 = DVE.mark(DVE.e.tensor_tensor(out=t[:], in0=vT[:, c, tb * 512:(tb + 1) * 512], in1=st["m"][:], op=ALU.subtract))
                    DVE.wait(e1)
                    e2 = DVE.mark(DVE.e.tensor_tensor(out=t[:], in0=t[:], in1=st["rstd"][:], op=ALU.mult))
                    ACT.wait(e2)
                    zdep = ACT.mark(ACT.e.activation(out=vT[:, c, tb * 512:(tb + 1) * 512], in_=t[:], func=AF.Silu,
                                                     scale=lng[:, c:c + 1], bias=lnb[:, c:c + 1]))
                    tmp_free[ti % 3] = zdep
                    ti += 1
                prev_tb_done = e2
            barrier()
            phs.close(); phs = contextlib.ExitStack()
            gb = k.sb("gb", [128, KC], F32, phs)
            gdep = DVE.mark(DVE.e.tensor_tensor(out=gb[:], in0=MOD(l, 2), in1=V(f"pw2b{i}"), op=ALU.mult))
            ACT.wait(gdep)

            def wload2(t, slot):
                sid = wring_slot_of(slot)
                sv = slot[:, 0:KC * 128].rearrange("p (kc n) -> p kc n", kc=KC)
                d1 = POOL.dma(sv, WT("conv_pw2_w")[i][:, t * 128:(t + 1) * 128].rearrange("(kc p) n -> p kc n", p=128), wring.dsems[sid])
                return [d1], [sv]

            pre, epi = make_res_epi(MOD(l, 2), gb, 0)
            run_gemm(range(KC), 1, KC, wload2, lambda kc, tb: vT[:, kc, tb * 512:(tb + 1) * 512], range(4), epi, pre=pre)
            finish_stores()
            barrier()
            phs.close()
        return False

    def ffn_layer(l):
        j = l // 2
        moe = (l % 2 == 1)
        if l == 0:
            nbanks[0] = 7
            mod_state["limit"] = mod_tiles_upto(2); mod_state["rate"] = 0.45; mod_state["acc"] = 0.0
        elif l == 1:
            mod_state["limit"] = mod_tiles_upto(5); mod_state["rate"] = 0.13; mod_state["acc"] = 0.0
        else:
            mod_state["rate"] = 0.0
        with contextlib.ExitStack() as ph:
            hTh = k.sb("hTh", [128, KC, 1024], BF16, ph)
            aT = k.sb("aT", [128, FC, 1024], BF16, ph)
            s_t = [k.sb(f"s{q}", [128, 512], BF16, ph) for q in range(2)]
            ffn_state.clear()
            if moe:
                rw = k.sb("rw", [128, KC, NE], F32, ph)
                rbias = k.sb("rbias", [NE, 1], F32, ph)
                rb0 = k.sb("rb0", [NE, 1], F32, ph)
                shf = k.sb("shf", [128, KC], F32, ph)
                lgT = k.sb("lgT", [NE, 1024], F32, ph)
                lg = k.sb("lg", [128, 8, NE], F32, ph)
                gates = k.sb("gates", [128, 8, NE], F32, ph)
                gT = k.sb("gT", [NE, 1024], BF16, ph)
                gate_b = [k.sb(f"gate_b{q}", [128, 1024], F32, ph) for q in range(2)]
                gate_free = [None, None]
                sel8 = [k.sb(f"sel8{q}", [NE, 128], BF16, ph) for q in range(2)]
                ones8 = k.sb("ones8", [NE, 128], F32, ph)
                sm = {n: k.sb(f"rt_{n}", [128, 8], F32, ph) for n in ["m1", "m2", "d", "e", "w1", "w2"]}
                big = {n: k.sb(f"rt_{n}", [128, 8, NE], F32, ph) for n in ["eq1", "l2", "eq2", "g1"]}
                d1 = SP.dma(rw[:], rw_d[j].rearrange("p (c e) -> p c e", e=NE), misc)
                d1 = SP.dma(rb0[:], rb_d[j], misc)
                DVE.wait(d1)
                DVE.e.memset(ones8[:], 1.0)
                PE.wait(d1)
                bb = bank_acquire()
                DVE.e.tensor_copy(out=shf[:], in_=MOD(l, 3))
                sdep = DVE.mark(DVE.e.tensor_copy(out=shf[:], in_=MOD(l, 3)))
                PE.wait(sdep)
                for c in range(KC):
                    ins = PE.e.matmul(psb[bb][0:NE, 0:1], lhsT=rw[:, c, :], rhs=shf[:, c:c + 1], start=(c == 0), stop=(c == KC - 1))
                pd = PE.mark(ins)
                DVE.wait(pd)
                bank_free[bb] = DVE.mark(DVE.e.tensor_tensor(out=rbias[:], in0=psb[bb][0:NE, 0:1], in1=rb0[:], op=ALU.add))
                ACT.wait(bank_free[bb])
            for half in range(2):
                tok0 = half * 1024
                with contextlib.ExitStack() as ph2:
                    router = dict(rw=rw, lgT=lgT, bias=rbias) if moe else None
                    phase_norm(ph2, V(f"nffn{l}"), MOD(l, 3), MOD(l, 4), hTh, tok0, 1024, router=router,
                               scratch=aT[:].rearrange("p f t -> p (f t)"))
                    barrier()
                if not moe:
                    ffn_half(ph, hTh, WT("ffn_w_gate")[j], WT("ffn_w_up")[j], WT("ffn_w_down")[j], MOD(l, 5), tok0, aT, s_t)
                else:
                    b = bank_acquire()
                    for tl in range(8):
                        ins = PE.e.transpose(out=psb[b][:, tl * NE:(tl + 1) * NE], in_=lgT[:, tl * 128:(tl + 1) * 128], identity=ident_f[0:NE, 0:NE])
                    pd = PE.mark(ins)
                    DVE.wait(pd)
                    c0 = DVE.mark(DVE.e.tensor_copy(out=lg[:], in_=psb[b][:, 0:8 * NE].rearrange("p (t e) -> p t e", e=NE)))
                    bank_free[b] = c0
                    DVE.wait(c0)

                    def bc(a):
                        return a[:, :].unsqueeze(2).broadcast_to([128, 8, NE])
                    x1 = DVE.mark(DVE.e.tensor_reduce(out=sm["m1"][:], in_=lg[:], axis=AX.X, op=ALU.max)); DVE.wait(x1)
                    x1 = DVE.mark(DVE.e.tensor_tensor(out=big["eq1"][:], in0=lg[:], in1=bc(sm["m1"]), op=ALU.is_equal)); DVE.wait(x1)
                    x1 = DVE.mark(DVE.e.scalar_tensor_tensor(out=big["l2"][:], in0=big["eq1"][:], scalar=-1e30, in1=lg[:], op0=ALU.mult, op1=ALU.add)); DVE.wait(x1)
                    x1 = DVE.mark(DVE.e.tensor_reduce(out=sm["m2"][:], in_=big["l2"][:], axis=AX.X, op=ALU.max)); DVE.wait(x1)
                    x1 = DVE.mark(DVE.e.tensor_tensor(out=big["eq2"][:], in0=big["l2"][:], in1=bc(sm["m2"]), op=ALU.is_equal)); DVE.wait(x1)
                    x1 = DVE.mark(DVE.e.tensor_tensor(out=sm["d"][:], in0=sm["m2"][:], in1=sm["m1"][:], op=ALU.subtract))
                    ACT.wait(x1)
                    x2 = ACT.mark(ACT.e.activation(out=sm["e"][:], in_=sm["d"][:], func=AF.Exp))
                    DVE.wait(x2)
                    x1 = DVE.mark(DVE.e.tensor_scalar(out=sm["w1"][:], in0=sm["e"][:], scalar1=1.0, scalar2=0.0, op0=ALU.add, op1=ALU.add)); DVE.wait(x1)
                    x1 = DVE.mark(DVE.e.reciprocal(out=sm["w1"][:], in_=sm["w1"][:])); DVE.wait(x1)
                    x1 = DVE.mark(DVE.e.tensor_tensor(out=sm["w2"][:], in0=sm["e"][:], in1=sm["w1"][:], op=ALU.mult)); DVE.wait(x1)
                    x1 = DVE.mark(DVE.e.tensor_tensor(out=big["g1"][:], in0=big["eq1"][:], in1=bc(sm["w1"]), op=ALU.mult)); DVE.wait(x1)
                    x1 = DVE.mark(DVE.e.tensor_tensor(out=gates[:], in0=big["eq2"][:], in1=bc(sm["w2"]), op=ALU.mult)); DVE.wait(x1)
                    x1 = DVE.mark(DVE.e.tensor_tensor(out=gates[:], in0=gates[:], in1=big["g1"][:], op=ALU.add))
                    PE.wait(x1)
                    bb2 = [bank_acquire(), bank_acquire()]
                    for tl in range(8):
                        ins = PE.e.transpose(out=psb[bb2[tl // 4]][0:NE, (tl % 4) * 128:(tl % 4 + 1) * 128], in_=gates[:, tl, :], identity=ident_f[:, :])
                    pd = PE.mark(ins)
                    DVE.wait(pd)
                    g0 = DVE.mark(DVE.e.tensor_copy(out=gT[:, 0:512], in_=psb[bb2[0]][0:NE, :]))
                    gtd = DVE.mark(DVE.e.tensor_copy(out=gT[:, 512:1024], in_=psb[bb2[1]][0:NE, :]))
                    bank_free[bb2[0]] = g0
                    bank_free[bb2[1]] = gtd
                    if stop_after == f"route_{l}" and half == 0:
                        POOL.wait(gtd)
                        POOL.dma(dbg_out[0:NE, 0:1024], gT[:], misc)
                        POOL.dma(dbg_out[NE:2 * NE, 0:1024], lgT[:], misc)
                        SP.wait((misc, misc.total))
                        return True
                    for e in range(NE):
                        q = e % 2
                        DVE.wait(gate_free[q], gtd)
                        sd = DVE.mark(DVE.e.tensor_scalar(out=sel8[q][:], in0=ones8[:], scalar1=ident_f[0:NE, e:e + 1], scalar2=0.0, op0=ALU.mult, op1=ALU.add))
                        PE.wait(sd)
                        gds = []
                        for tb in range(2):
                            b = bank_acquire()
                            pd = PE.mark(PE.e.matmul(psb[b][:, :], lhsT=sel8[q][:], rhs=gT[:, tb * 512:(tb + 1) * 512], start=True, stop=True))
                            ACT.wait(pd, gate_free[q])
                            gd = ACT.mark(ACT.e.activation(out=gate_b[q][:, tb * 512:(tb + 1) * 512], in_=psb[b][:, :], func=AF.Copy))
                            bank_free[b] = gd
                            gds.append(gd)
                        DVE.wait(gds)
                        ffn_half(ph, hTh, WT("moe_w_gate")[j, e], WT("moe_w_up")[j, e], WT("moe_w_down")[j, e], MOD(l, 5), tok0, aT, s_t, gate_b=gate_b[q])
                        gate_free[q] = [(DVE.sem, DVE.sem.total), (PE.sem, PE.sem.total)]
                if half == 1 and l == 0:
                    mod_flush(2)
                if half == 1 and l == 1:
                    mod_flush(5)
                    nbanks[0] = 8
                finish_stores()
                barrier()
        return False


    def kv_phase():
        with contextlib.ExitStack() as ph:
            hT = k.sb("hT", [128, KC, S], BF16, ph)
            with contextlib.ExitStack() as ph2:
                phase_norm(ph2, V("kvng"), KVMOD(0), KVMOD(1), hT, 0, S)
                barrier()
            wkvf = WT("w_kvf")
            stg = [k.sb(f"kst{q}", [128, 512], BF16, ph) for q in range(3)]
            stg_ring = Ring(k, "kst", [t[:] for t in stg], es=ph)
            cnt = [0]

            def evac(out, in_, waits):
                E = ACT if cnt[0] % 2 == 0 else DVE
                cnt[0] += 1
                E.wait(*waits)
                if E is ACT:
                    return ACT.mark(ACT.e.activation(out=out, in_=in_, func=AF.Copy))
                return DVE.mark(DVE.e.tensor_copy(out=out, in_=in_))

            def wloadk(t, slot):
                sid = wring_slot_of(slot)
                sv = slot[:, 0:KC * 128].rearrange("p (kc n) -> p kc n", kc=KC)
                d1 = POOL.dma(sv, wkvf[:, t * 128:(t + 1) * 128].rearrange("(kc p) n -> p kc n", p=128), wring.dsems[sid])
                return [d1], [sv]

            def epik(t, tb, banks, rdy):
                s = stg_ring.next()
                c = evac(stg[s][:], psb[banks[0]][:, :], [rdy[0], stg_ring.free[s]])
                SP.wait(c)
                stg_ring.free[s] = SP.dma(KTs[t * 128:(t + 1) * 128, tb * 512:(tb + 1) * 512], stg[s][:], stg_ring.dsems[s])
                return [c]

            run_gemm(range(H), 1, KC, wloadk, lambda kc, tb: hT[:, kc, tb * 512:(tb + 1) * 512], range(4), epik)
            vst = [k.sb(f"vst{q}", [128, 256], BF16, ph) for q in range(3)]
            vring = Ring(k, "vst", [t[:] for t in vst], es=ph)
            for cb in range(8):
                s = wring.next()
                POOL.wait(wring.free[s])
                sv = wring.views[s][:, 0:KC * 256].rearrange("p (kc n) -> p kc n", kc=KC)
                dep = POOL.dma(sv, wkvf[:, D + cb * 256: D + (cb + 1) * 256].rearrange("(kc p) n -> p kc n", p=128), wring.dsems[s])
                PE.wait(dep)
                for tt in range(16):
                    b = bank_acquire()
                    for kc in range(KC):
                        ins = PE.e.matmul(psb[b][:, 0:256], lhsT=hT[:, kc, tt * 128:(tt + 1) * 128], rhs=sv[:, kc, :],
                                          start=(kc == 0), stop=(kc == KC - 1))
                    rdy = PE.mark(ins)
                    q = vring.next()
                    c = evac(vst[q][:], psb[b][:, 0:256], [rdy, vring.free[q]])
                    bank_free[b] = c
                    SP.wait(c)
                    vring.free[q] = SP.dma(Vs[tt * 128:(tt + 1) * 128, cb * 256:(cb + 1) * 256], vst[q][:], vring.dsems[q])
                wring.free[s] = rdy
            wf = k.sb("wf", [128, KC, H], BF16, ph)
            dwf = POOL.dma(wf[:], wkvf[:, 2 * D:2 * D + H].rearrange("(kc p) n -> p kc n", p=128), misc)
            bft = k.sb("bft", [H, 1], F32, ph)
            nbf = k.sb("nbf", [H, 1], F32, ph)
            sp = k.sb("sp", [H, S], F32, ph)
            ones16 = k.sb("ones16", [H, S], F32, ph)
            PT = k.sb("PT", [H, S], F32, ph)
            d = SP.dma(bft[:], bf_d[:, :], misc)
            DVE.wait(d)
            DVE.e.memset(ones16[:], 1.0)
            nb = DVE.mark(DVE.e.tensor_scalar(out=nbf[:], in0=bft[:], scalar1=-1.0, scalar2=0.0, op0=ALU.mult, op1=ALU.add))
            PE.wait(dwf)
            for tb in range(4):
                b = bank_acquire()
                for kc in range(KC):
                    ins = PE.e.matmul(psb[b][0:H, :], lhsT=wf[:, kc, :], rhs=hT[:, kc, tb * 512:(tb + 1) * 512],
                                      start=(kc == 0), stop=(kc == KC - 1))
                rdy = PE.mark(ins)
                ACT.wait(rdy, nb)
                e1 = ACT.mark(ACT.e.activation(out=sp[:, tb * 512:(tb + 1) * 512], in_=psb[b][0:H, :], func=AF.Exp,
                                               bias=nbf[:, 0:1], scale=-1.0))
                bank_free[b] = e1
                ACT.wait(e1)
                e2 = ACT.mark(ACT.e.activation(out=sp[:, tb * 512:(tb + 1) * 512], in_=sp[:, tb * 512:(tb + 1) * 512], func=AF.Ln,
                                               bias=1.0, scale=1.0))
            DVE.wait(e2)
            scn = DVE.mark(DVE.e.tensor_tensor_scan(out=PT[:], data0=ones16[:], data1=sp[:], initial=0.0, op0=ALU.mult, op1=ALU.add))
            DVE.wait(scn)
            DVE.mark(DVE.e.tensor_scalar(out=GqT[:], in0=PT[:], scalar1=-float(np.sqrt(HD)), scalar2=0.0, op0=ALU.mult, op1=ALU.add))
            PE.wait(scn)
            b = bank_acquire()
            for tt in range(16):
                ins = PE.e.transpose(out=psb[b][:, tt * H:(tt + 1) * H], in_=PT[:, tt * 128:(tt + 1) * 128], identity=ident_f[0:H, 0:H])
            rdy = PE.mark(ins)
            DVE.wait(rdy)
            bank_free[b] = DVE.mark(DVE.e.tensor_copy(out=negF[:], in_=psb[b][:, 0:16 * H]))
            for r_ in (stg_ring, vring):
                for q in range(3):
                    SP.wait((r_.dsems[q], r_.dsems[q].total))
            if stop_after == "kv":
                POOL.e.wait_ge(bar.h, bar.total)
                barrier()
                POOL.e.wait_ge(bar.h, bar.total)
                POOL.dma(dbg_out[0:H, :], PT[:], misc)
                POOL.dma(dbg_out[128:256, 0:256], negF[:], misc)
                POOL.dma(dbg_out[256:256 + H, :], GqT[:], misc)
                SP.wait((misc, misc.total))
                return True
            barrier()
        return False

    def attn_layer(l):
        i = l - 2
        with contextlib.ExitStack() as ph:
            hT = k.sb("hT", [128, KC, S], BF16, ph)
            with contextlib.ExitStack() as ph2:
                phase_norm(ph2, V(f"nmix{l}"), MOD(l, 0), MOD(l, 1), hT, 0, S)
                barrier()
            qT = k.sb("qT", [128, H, S], BF16, ph)
            cnt = [0]

            def wloadq(t, slot):
                sid = wring_slot_of(slot)
                sv = slot[:, 0:KC * 128].rearrange("p (kc n) -> p kc n", kc=KC)
                d1 = POOL.dma(sv, WT("attn_wq")[i][:, t * 128:(t + 1) * 128].rearrange("(kc p) n -> p kc n", p=128), wring.dsems[sid])
                return [d1], [sv]

            def epiq(t, tb, banks, rdy):
                E = ACT if cnt[0] % 2 == 0 else DVE
                cnt[0] += 1
                E.wait(rdy[0])
                if E is ACT:
                    c = ACT.mark(ACT.e.activation(out=qT[:, t, tb * 512:(tb + 1) * 512], in_=psb[banks[0]][:, :], func=AF.Copy))
                else:
                    c = DVE.mark(DVE.e.tensor_copy(out=qT[:, t, tb * 512:(tb + 1) * 512], in_=psb[banks[0]][:, :]))
                return [c]

            run_gemm(range(H), 1, KC, wloadq, lambda kc, tb: hT[:, kc, tb * 512:(tb + 1) * 512], range(4), epiq)
            barrier()
            oT = hT
            phs = contextlib.ExitStack()
            KT = [k.sb(f"KT{q}", [128, S], BF16, phs) for q in range(2)]
            Vh = [k.sb(f"Vh{q}", [128, 16, 128], BF16, phs) for q in range(2)]
            kvring = Ring(k, "kv", [None, None], es=phs)
            pT = [k.sb(f"pT{q}", [128, 512], BF16, phs) for q in range(4)]
            pT_free = [None] * 4
            pi = [0]
            selh = [k.sb(f"selh{q}", [H, 128], BF16, phs) for q in range(2)]
            sel_free = [None, None]
            ones16b = k.sb("ones16b", [H, 128], F32, phs)
            rd = [k.sb(f"rd{q}", [128, 512], F32, phs) for q in range(2)]
            rd_free = [None, None]
            ri = [0]
            o1 = DVE.mark(DVE.e.memset(ones16b[:], 1.0))
            DVE.wait(o1)
            scale = float(HD ** -0.5)
            acc_i = [0]; sc_i = [0]
            for h in range(H):
                s = kvring.next()
                SP.wait(kvring.free[s])
                d1 = SP.dma(KT[s][:], KTs[h * 128:(h + 1) * 128, :], kvring.dsems[s])
                d2 = SP.dma(Vh[s][:], Vs[:, h * 128:(h + 1) * 128].rearrange("(i p) d -> p i d", p=128), kvring.dsems[s])
                q2 = h % 2
                DVE.wait(sel_free[q2])
                sd = DVE.mark(DVE.e.tensor_scalar(out=selh[q2][:], in0=ones16b[:], scalar1=ident_f[0:H, h:h + 1], scalar2=0.0,
                                                  op0=ALU.mult, op1=ALU.add))
                PE.wait(d1, d2, sd)
                fin = None
                for j in range(4):
                    acc_i[0] += 1
                    bo, bd = (0, 1) if acc_i[0] % 2 == 0 else (2, 3)
                    PE.wait(bank_free[bo], bank_free[bd])
                    ntile = 4 * (j + 1)

                    def qk(ii):
                        o = max(0, ii - 4 * j)
                        c0 = 128 * o
                        sc_i[0] += 1
                        b = 4 + sc_i[0] % 4
                        PE.wait(bank_free[b])
                        PE.e.matmul(psb[b][:, c0:512], lhsT=KT[s][:, ii * 128:(ii + 1) * 128], rhs=qT[:, h, j * 512 + c0:(j + 1) * 512],
                                    start=True, stop=False)
                        ins = PE.e.matmul(psb[b][:, c0:512], lhsT=selh[q2][:], rhs=GqT[:, j * 512 + c0:(j + 1) * 512], start=False, stop=True)
                        return b, c0, PE.mark(ins)

                    pend = []
                    nxt = 0
                    while nxt < min(3, ntile):
                        pend.append(qk(nxt))
                        nxt += 1
                    for ii in range(ntile):
                        b, c0, rdy = pend.pop(0)
                        if nxt < ntile:
                            pend.append(qk(nxt))
                            nxt += 1
                        p = pi[0] % 4
                        pi[0] += 1
                        ACT.wait(rdy, pT_free[p])
                        e = ACT.mark(ACT.e.activation(out=pT[p][:, c0:512], in_=psb[b][:, c0:512], func=AF.Exp,
                                                      bias=negF[:, ii * H + h: ii * H + h + 1], scale=scale))
                        bank_free[b] = e
                        dep = e
                        if ii >= 4 * j:
                            DVE.wait(e)
                            dep = DVE.mark(DVE.e.tensor_tensor(out=pT[p][:, c0:c0 + 128], in0=pT[p][:, c0:c0 + 128], in1=tri_b[:], op=ALU.mult))
                        PE.wait(dep)
                        PE.e.matmul(psb[bo][:, c0:512], lhsT=Vh[s][:, ii, :], rhs=pT[p][:, c0:512], start=(ii == 0), stop=(ii == ntile - 1))
                        ins = PE.e.matmul(psb[bd][:, c0:512], lhsT=ones_b[:], rhs=pT[p][:, c0:512], start=(ii == 0), stop=(ii == ntile - 1))
                        pT_free[p] = PE.mark(ins)
                    fin = pT_free[p]
                    r = ri[0] % 2
                    ri[0] += 1
                    DVE.wait(fin, rd_free[r])
                    r1 = DVE.mark(DVE.e.reciprocal(out=rd[r][:], in_=psb[bd][:, :]))
                    DVE.wait(r1)
                    r2 = DVE.mark(DVE.e.tensor_tensor(out=oT[:, h, j * 512:(j + 1) * 512], in0=psb[bo][:, :], in1=rd[r][:], op=ALU.mult))
                    rd_free[r] = r2
                    bank_free[bo] = r2
                    bank_free[bd] = r1
                kvring.free[s] = fin
                sel_free[q2] = fin
            barrier()
            phs.close()
            if stop_after == f"att_{l}":
                POOL.e.wait_ge(bar.h, bar.total)
                POOL.dma(dbg_out[:, :].rearrange("(c p) t -> p c t", p=128), oT[:, :, :], misc)
                SP.wait((misc, misc.total))
                return True

            def wloado(t, slot):
                sid = wring_slot_of(slot)
                sv = slot[:, 0:KC * 128].rearrange("p (kc n) -> p kc n", kc=KC)
                d1 = POOL.dma(sv, WT("attn_wo")[i][:, t * 128:(t + 1) * 128].rearrange("(kc p) n -> p kc n", p=128), wring.dsems[sid])
                return [d1], [sv]

            pre, epi = make_res_epi(MOD(l, 2), None, 0)
            run_gemm(range(KC), 1, KC, wloado, lambda kc, tb: oT[:, kc, tb * 512:(tb + 1) * 512], range(4), epi, pre=pre)
            finish_stores()
            barrier()
        return False

    def final_phase(outT):
        with contextlib.ExitStack() as ph:
            zer = k.sb("zer", [128, KC], F32, ph)
            z0 = DVE.mark(DVE.e.memset(zer[:], 0.0))
            DVE.wait(z0)
            last = phase_norm(ph, V("fng"), None, zer[:], None, 0, S, out_dram=outT)
            SP.wait(last)
            for q in range(3):
                pass
        return last

    def run_seq():
        outT = outT_all[cur[0]]
        seq_setup()
        phase_mod()
        barrier()
        if stop_after == "mod":
            SP.dma(dbg_out[:, 0:416], modall[:, :], misc)
            SP.wait((misc, misc.total))
            return True
        done = False
        if stop_after == "only_attn":
            assert not kv_phase()
            assert not attn_layer(2)
            finish_stores()
            barrier()
            d = SP.dma(outT, xTs[:, :], misc)
            SP.wait(d)
            return True
        for l in range(4):
            if l == 2:
                if kv_phase():
                    return True
            r_ = conv_layer(l) if l < 2 else attn_layer(l)
            if r_:
                return True
            if stop_after == f"mix_{l}":
                done = True
                break
            if ffn_layer(l):
                return True
            if stop_after == f"ffn_{l}":
                done = True
                break
        finish_stores()
        barrier()
        if done:
            d = SP.dma(outT, xTs[:, :], misc)
            SP.wait(d)
            return True
        final_phase(outT)
        barrier()
        return False

    for si in range(nseq):
        cur[0] = si
        if run_seq():
            return


def _prep_common(inp):
    g = lambda n: np.asarray(inp[n], np.float32)
    dww = np.zeros((2, 128, CW * 16), np.float32)
    for i in range(2):
        for kk in range(CW):
            dww[i][:, kk * 16:(kk + 1) * 16] = _fm(g("conv_dw_w")[i, kk])
    rw = np.zeros((2, 128, KC * NE), np.float32)
    for j in range(2):
        rw[j] = g("moe_router_w")[j].reshape(KC, 128, NE).transpose(1, 0, 2).reshape(128, KC * NE)
    rb = g("moe_router_b").reshape(2, NE, 1).copy()
    common = dict(
        dww=dww, bf=g("b_f").reshape(H, 1).copy(), rw=rw, rb=rb,
        ident=np.eye(128, dtype=np.float32), tri=np.triu(np.ones((128, 128), np.float32)),
        ada_w=g("ada_w"), conv_pw1_w=g("conv_pw1_w"), conv_pw2_w=g("conv_pw2_w"), kv_ada_w=g("kv_ada_w"),
        w_kvf=g("w_kvf"), attn_wq=g("attn_wq"), attn_wo=g("attn_wo"), ffn_w_gate=g("ffn_w_gate"),
        ffn_w_up=g("ffn_w_up"), ffn_w_down=g("ffn_w_down"), moe_w_gate=g("moe_w_gate"),
        moe_w_up=g("moe_w_up"), moe_w_down=g("moe_w_down"),
    )
    return common


def _vecs_for(inp, b):
    g = lambda n: np.asarray(inp[n], np.float32)
    v = np.zeros((128, NV * 16), np.float32)

    def put(name, vec):
        i = VIDX[name]
        v[:, i * 16:(i + 1) * 16] = _fm(vec)
    put("c", g("c")[b])
    for l in range(4):
        for s in range(6):
            put(f"ab{l}_{s}", g("ada_b")[l, s * D:(s + 1) * D])
        put(f"nmix{l}", g("norm_mix_g")[l]); put(f"nffn{l}", g("norm_ffn_g")[l])
    put("kvab_0", g("kv_ada_b")[:D]); put("kvab_1", g("kv_ada_b")[D:])
    for i in range(2):
        put(f"pw1bv{i}", g("conv_pw1_b")[i, :D]); put(f"pw1bg{i}", g("conv_pw1_b")[i, D:])
        put(f"dwb{i}", g("conv_dw_b")[i]); put(f"lng{i}", g("conv_ln_g")[i]); put(f"lnb{i}", g("conv_ln_b")[i])
        put(f"pw2b{i}", g("conv_pw2_b")[i])
    put("kvng", g("kv_norm_g")); put("fng", g("final_norm_g"))
    return v


def make_in_maps(inp, seqs_per_core, names=None):
    common = _prep_common(inp)
    if names is not None:
        common = {k_: v_ for k_, v_ in common.items() if k_ in names}
    x = np.asarray(inp["x"], np.float32)
    maps = []
    for seqs in seqs_per_core:
        m = dict(common)
        m["xT"] = np.stack([np.ascontiguousarray(x[b].T) for b in seqs], 0)
        m["vecs"] = np.stack([_vecs_for(inp, b) for b in seqs], 0)
        maps.append(m)
    return maps


NUSED = 8
NSEQ = 8 // NUSED


def kernel(**inputs):
    nc = build(nseq=NSEQ)
    seqs = [list(range(c * NSEQ, (c + 1) * NSEQ)) for c in range(NUSED)]
    in_maps = make_in_maps(inputs, seqs)
    res = run_bass_kernel_spmd(nc, in_maps, core_ids=list(range(NUSED)))
    out = np.zeros((8, S, D), np.float32)
    for c in range(NUSED):
        o = np.asarray(res.results[c]["outT"])
        for i, b in enumerate(seqs[c]):
            out[b] = o[i].T
    return out
```

```python
import contextlib
import numpy as np
import concourse.bass as bass
import concourse.mybir as mybir
from concourse.bass_utils import run_bass_kernel_spmd

F32, BF16 = mybir.dt.float32, mybir.dt.bfloat16
AF = mybir.ActivationFunctionType
ALU = mybir.AluOpType
AX = mybir.AxisListType

D = 2048; S = 2048; H = 16; HD = 128; CW = 31; DFF = 5632; NE = 8
KC = D // 128; FC = DFF // 128
EPS = 1e-6
NCORES = 8
WSLOT = FC * 128

VEC_NAMES = ["c"]
for _l in range(4):
    VEC_NAMES += [f"ab{_l}_{s}" for s in range(6)]
VEC_NAMES += ["kvab_0", "kvab_1"]
for _l in range(4):
    VEC_NAMES += [f"nmix{_l}", f"nffn{_l}"]
for _i in range(2):
    VEC_NAMES += [f"pw1bv{_i}", f"pw1bg{_i}", f"dwb{_i}", f"lng{_i}", f"lnb{_i}", f"pw2b{_i}"]
VEC_NAMES += ["kvng", "fng"]
VIDX = {n: i for i, n in enumerate(VEC_NAMES)}
NV = len(VEC_NAMES)


def _fm(v):
    return np.ascontiguousarray(np.asarray(v, np.float32).reshape(KC, 128).T)


class Sem:
    def __init__(self, h):
        self.h = h
        self.total = 0


class Eng:
    def __init__(self, k, e, name):
        self.k = k; self.e = e; self.name = name
        self.sem = k.newsem("p_" + name)
        self.seen = {}

    def wait(self, *deps):
        for d in deps:
            if d is None:
                continue
            if isinstance(d, list):
                self.wait(*d)
                continue
            sem, v = d
            if self.seen.get(id(sem), 0) >= v:
                continue
            self.e.wait_ge(sem.h, v)
            self.seen[id(sem)] = v

    def mark(self, ins):
        ins.then_inc(self.sem.h, 1)
        self.sem.total += 1
        return (self.sem, self.sem.total)

    def dma(self, out, in_, dsem):
        ins = self.e.dma_start(out=out, in_=in_)
        ins.then_inc(dsem.h, 16)
        dsem.total += 16
        return (dsem, dsem.total)


class Ring:
    def __init__(self, k, name, views, dma=True, es=None):
        self.views = views; self.n = len(views)
        self.dsems = [k.newsem(f"{name}{i}", es) for i in range(self.n)] if dma else None
        self.free = [None] * self.n
        self.i = 0

    def next(self):
        s = self.i % self.n
        self.i += 1
        return s


class K:
    def __init__(self):
        self.nc = bass.Bass("TRN2", target_bir_lowering=False)
        self.es = contextlib.ExitStack()
        self.nsem = 0

    def newsem(self, name, es=None):
        pool = self.__dict__.setdefault("sempool", [])
        if es is not None and pool:
            sem = pool.pop()
        else:
            self.nsem += 1
            sem = Sem(self.es.enter_context(self.nc.semaphore(f"{name}_{self.nsem}")))
        if es is not None:
            es.callback(pool.append, sem)
        return sem

    def sb(self, name, shape, dt, es=None):
        self.nsb = getattr(self, "nsb", 0) + 1
        return (es or self.es).enter_context(self.nc.sbuf_tensor(f"{name}_s{self.nsb}", list(shape), dt))


def build(stop_after=None, dbg=None, nseq=1):
    k = K()
    nc = k.nc
    with k.es:
        _build(k, nc, stop_after, dbg, nseq)
    return nc


def _build(k, nc, stop_after, dbg, nseq):
    def din(name, shape, dt=F32):
        return nc.dram_tensor(name, list(shape), dt, kind="ExternalInput").ap()

    xT_in = din("xT", [nseq, D, S])
    vecs_d = din("vecs", [nseq, 128, NV * 16])
    cur = [0]
    dww_d = din("dww", [2, 128, CW * 16])
    bf_d = din("bf", [16, 1])
    rw_d = din("rw", [2, 128, KC * NE])
    rb_d = din("rb", [2, NE, 1])
    ident_d = din("ident", [128, 128])
    tri_d = din("tri", [128, 128])
    WSHAPES = dict(ada_w=[4, D, 6 * D], conv_pw1_w=[2, D, 2 * D], conv_pw2_w=[2, D, D], kv_ada_w=[D, 2 * D],
                   w_kvf=[D, 2 * D + H], attn_wq=[2, D, D], attn_wo=[2, D, D], ffn_w_gate=[2, D, DFF],
                   ffn_w_up=[2, D, DFF], ffn_w_down=[2, DFF, D], moe_w_gate=[2, NE, D, DFF],
                   moe_w_up=[2, NE, D, DFF], moe_w_down=[2, NE, DFF, D])
    wcache = {}

    def WT(name):
        if name not in wcache:
            wcache[name] = din(name, WSHAPES[name])
        return wcache[name]
    outT_all = nc.dram_tensor("outT", [nseq, D, S], F32, kind="ExternalOutput").ap()
    dbg_out = None
    if dbg is not None:
        dbg_out = nc.dram_tensor("dbg", list(dbg), F32, kind="ExternalOutput").ap()
    xTs = nc.dram_tensor("xTs", [D, S], F32, kind="Internal").ap()
    KTs = nc.dram_tensor("KTs", [D, S], BF16, kind="Internal").ap()
    Vs = nc.dram_tensor("Vs", [S, D], BF16, kind="Internal").ap()

    PE = Eng(k, nc.tensor, "pe"); ACT = Eng(k, nc.scalar, "act"); DVE = Eng(k, nc.vector, "dve")
    POOL = Eng(k, nc.gpsimd, "pool"); SP = Eng(k, nc.sync, "sp")
    bar = k.newsem("bar")
    BAR_ENGS = [PE, ACT, DVE, SP]

    def barrier():
        for e in BAR_ENGS:
            e.e.drain().then_inc(bar.h, 1)
        bar.total += len(BAR_ENGS)
        for e in BAR_ENGS:
            e.e.wait_ge(bar.h, bar.total)

    vecs = k.sb("vecs", [128, NV * 16], F32)
    modall = k.sb("modall", [128, 4 * 96 + 32], F32)
    ident_f = k.sb("ident_f", [128, 128], F32)
    ident_b = k.sb("ident_b", [128, 128], BF16)
    tri_b = k.sb("tri_b", [128, 128], BF16)
    ones_b = k.sb("ones_b", [128, 128], BF16)
    cact = k.sb("cact", [128, KC], BF16)
    eps_c = k.sb("eps_c", [128, 1], F32)
    deps_c = k.sb("deps_c", [128, 1], F32)
    negF = k.sb("negF", [128, 16 * H], F32)
    GqT = k.sb("GqT", [H, S], BF16)
    wring_t = k.sb("wring", [128, 3 * WSLOT], BF16)
    wring = Ring(k, "w", [wring_t[:, i * WSLOT:(i + 1) * WSLOT] for i in range(3)])
    psb = [k.es.enter_context(nc.psum_tensor(f"ps{i}", [128, 512], F32)) for i in range(8)]
    bank_free = [None] * 8
    bank_i = [0]
    misc = k.newsem("misc")

    def V(name):
        i = VIDX[name]
        return vecs[:, i * 16:(i + 1) * 16]

    def MOD(l, s):
        return modall[:, l * 96 + s * 16: l * 96 + (s + 1) * 16]

    def KVMOD(s):
        return modall[:, 384 + s * 16: 384 + (s + 1) * 16]

    nbanks = [8]

    def bank_acquire():
        b = bank_i[0] % nbanks[0]
        bank_i[0] += 1
        PE.wait(bank_free[b])
        return b

    def selfwait(E, dep):
        E.wait(dep)

    d0 = SP.dma(ident_f[:], ident_d[:, :], misc)
    tri_f = k.sb("tri_f", [128, 128], F32)
    d0 = SP.dma(tri_f[:], tri_d[:, :], misc)
    DVE.wait(d0)
    DVE.e.tensor_copy(out=ident_b[:], in_=ident_f[:])
    DVE.e.tensor_copy(out=tri_b[:], in_=tri_f[:])
    DVE.e.memset(eps_c[:], float(EPS))
    DVE.e.memset(deps_c[:], float(D * EPS))
    m = DVE.mark(DVE.e.memset(ones_b[:], 1.0))
    PE.wait(m); ACT.wait(m); SP.wait(m)
    barrier()

    def seq_setup():
        dv = SP.dma(vecs[:], vecs_d[cur[0]], misc)
        dx = SP.dma(xTs[:, :], xT_in[cur[0]], misc)
        ACT.wait(dv)
        cdep = ACT.mark(ACT.e.activation(out=cact[:], in_=V("c"), func=AF.Silu))
        SP.wait(dx)
        PE.wait(cdep); DVE.wait(dv)
        barrier()

    mod_groups = []
    mod_state = dict(pos=0, acc=0.0, rate=0.0, tiles=[], limit=0)

    def mod_init():
        mod_groups.clear()
        mod_groups.extend([(WT("ada_w")[l], 96, l * 96, VIDX[f"ab{l}_0"]) for l in range(4)])
        mod_groups.insert(2, (WT("kv_ada_w"), 32, 384, VIDX["kvab_0"]))
        mod_state["tiles"] = [(gi, j2) for gi, g in enumerate(mod_groups) for j2 in range(g[1] // 2)]
        mod_state["pos"] = 0

    def mod_emit_tile(b):
        gi, j2 = mod_state["tiles"][mod_state["pos"]]
        mod_state["pos"] += 1
        (w, nch, off, vi) = mod_groups[gi]
        if j2 == 0:
            PE.wait(bank_free[b])
        s = wring.next()
        POOL.wait(wring.free[s])
        sv = wring.views[s][:, 0:KC * 256].rearrange("p (kc n) -> p kc n", kc=KC)
        dep = POOL.dma(sv, w[:, j2 * 256:(j2 + 1) * 256].rearrange("(kc p) n -> p kc n", p=128), wring.dsems[s])
        PE.wait(dep)
        for jj in range(2):
            j = j2 * 2 + jj
            for kc in range(KC):
                ins = PE.e.matmul(psb[b][:, j:j + 1], lhsT=sv[:, kc, jj * 128:(jj + 1) * 128], rhs=cact[:, kc:kc + 1],
                                  start=(kc == 0), stop=(kc == KC - 1))
        wring.free[s] = PE.mark(ins)
        if j2 == nch // 2 - 1:
            DVE.wait(wring.free[s])
            bank_free[b] = DVE.mark(DVE.e.tensor_tensor(out=modall[:, off:off + nch], in0=psb[b][:, 0:nch],
                                                        in1=vecs[:, vi * 16: vi * 16 + nch], op=ALU.add))

    def mod_tiles_upto(ngroups):
        return sum(g[1] // 2 for g in mod_groups[:ngroups])

    def phase_mod():
        mod_init()
        b = bank_acquire()
        while mod_state["pos"] < mod_tiles_upto(1):
            mod_emit_tile(b)

    def mod_side():
        mod_state["acc"] += mod_state["rate"]
        while mod_state["acc"] >= 1.0:
            mod_state["acc"] -= 1.0
            if mod_state["pos"] < mod_state["limit"]:
                mod_emit_tile(7)

    def mod_flush(ngroups):
        while mod_state["pos"] < mod_tiles_upto(ngroups):
            mod_emit_tile(7)

    def phase_norm(ph, g_ap, sh_ap, sc_ap, hT, tok0, ntok, router=None, out_dram=None, scratch=None):
        off = [0]

        def alloc(name, shape, dt):
            if scratch is None:
                return k.sb(name, shape, dt, ph)
            n = int(np.prod(shape[1:])) * (2 if dt == F32 else 1)
            v = scratch[:, off[0]:off[0] + n]
            off[0] += n
            if dt == F32:
                v = v.bitcast(F32)
            if len(shape) == 3:
                v = v.rearrange("p (a b) -> p a b", a=shape[1])
            return v
        avec = k.sb("avec", [128, KC], F32, ph)
        xt = [alloc(f"xt{i}", [128, KC, 512], F32) for i in range(2)]
        xring = Ring(k, "nx", [t[:] for t in xt], es=ph)
        sq = [alloc(f"sq{i}", [128, 4, 512], BF16) for i in range(2)]
        sq_free = [None, None]
        rstd = [alloc(f"rstd{i}", [128, 512], F32) for i in range(2)]
        tmp = [alloc(f"ntmp{i}", [128, 512], F32) for i in range(3)]
        tmp_free = [None] * 3
        rstd_free = [None, None]
        fin_sems = [k.newsem(f"fin{q}", ph) for q in range(3)] if out_dram is not None else None
        a0 = DVE.mark(DVE.e.scalar_tensor_tensor(out=avec[:], in0=sc_ap, scalar=1.0, in1=g_ap, op0=ALU.add, op1=ALU.mult))
        DVE.wait(a0)
        adep = DVE.mark(DVE.e.tensor_scalar(out=avec[:], in0=avec[:], scalar1=float(np.sqrt(D)), scalar2=0.0, op0=ALU.mult, op1=ALU.add))
        DVE.wait(adep)
        nt = ntok // 512
        ti = 0
        last = None
        for tb in range(nt):
            s = xring.next()
            SP.wait(xring.free[s])
            xdep = SP.dma(xring.views[s], xTs[:, tok0 + tb * 512: tok0 + (tb + 1) * 512].rearrange("(c p) t -> p c t", p=128),
                          xring.dsems[s])
            x = xt[s]
            b = bank_acquire()
            ACT.wait(xdep)
            for g4 in range(4):
                q = (tb * 4 + g4) % 2
                ACT.wait(sq_free[q])
                sdep = ACT.mark(ACT.e.activation(out=sq[q][:], in_=x[:, g4 * 4:(g4 + 1) * 4, :], func=AF.Square))
                PE.wait(sdep)
                for c4 in range(4):
                    c = g4 * 4 + c4
                    ins = PE.e.matmul(psb[b][:, :], lhsT=ones_b[:], rhs=sq[q][:, c4, :], start=(c == 0), stop=(c == KC - 1))
                sq_free[q] = PE.mark(ins)
            r = rstd[tb % 2]
            ACT.wait(sq_free[q], rstd_free[tb % 2])
            r0 = ACT.mark(ACT.e.activation(out=r[:], in_=psb[b][:, :], func=AF.Sqrt, bias=deps_c[:, 0:1], scale=1.0))
            bank_free[b] = r0
            DVE.wait(r0)
            rdep = DVE.mark(DVE.e.reciprocal(out=r[:], in_=r[:]))
            DVE.wait(rdep, xdep)
            rb_bank = None
            if router is not None:
                rb_bank = bank_acquire()
            for c in range(KC):
                t = tmp[ti % 3]
                DVE.wait(tmp_free[ti % 3])
                tdep = DVE.mark(DVE.e.scalar_tensor_tensor(out=t[:], in0=x[:, c, :], scalar=avec[:, c:c + 1], in1=r[:],
                                                           op0=ALU.mult, op1=ALU.mult))
                if out_dram is not None:
                    SP.wait(tdep)
                    fdep = SP.dma(out_dram[c * 128:(c + 1) * 128, tok0 + tb * 512: tok0 + (tb + 1) * 512], t[:], fin_sems[ti % 3])
                    tmp_free[ti % 3] = fdep
                    ti += 1
                    last = fdep
                    continue
                ACT.wait(tdep)
                fdep = ACT.mark(ACT.e.activation(out=hT[:, c, tb * 512:(tb + 1) * 512], in_=t[:], func=AF.Identity,
                                                 bias=sh_ap[:, c:c + 1], scale=1.0))
                if router is not None:
                    PE.wait(tdep)
                    ins = PE.e.matmul(psb[rb_bank][0:NE, :], lhsT=router["rw"][:, c, :], rhs=t[:], start=(c == 0), stop=(c == KC - 1))
                    pdep = PE.mark(ins)
                    tmp_free[ti % 3] = [fdep, pdep]
                else:
                    tmp_free[ti % 3] = fdep
                ti += 1
                last = fdep
            xring.free[s] = [tdep]
            rstd_free[tb % 2] = tdep
            if router is not None:
                ACT.wait(pdep)
                bank_free[rb_bank] = ACT.mark(ACT.e.activation(out=router["lgT"][:, tb * 512:(tb + 1) * 512], in_=psb[rb_bank][0:NE, :],
                                                               func=AF.Identity, bias=router["bias"][:, 0:1], scale=1.0))
        return last

    def run_gemm(steps, n_sub, kc_n, wload, rhs, tbs, epi, pre=None, ahead=3, side=None):
        steps = list(steps); tbs = list(tbs)
        its = [(t_, tb_) for t_ in steps for tb_ in tbs]
        if pre is not None:
            for i_ in range(min(ahead, len(its))):
                pre(*its[i_])
        idx = 0
        for t in steps:
            s = wring.next()
            POOL.wait(wring.free[s])
            deps, subs = wload(t, wring.views[s])
            PE.wait(deps)
            for tb in tbs:
                if pre is not None and idx + ahead < len(its):
                    pre(*its[idx + ahead])
                idx += 1
                banks = [bank_acquire() for _ in range(n_sub)]
                rdy = []
                for si in range(n_sub):
                    for kc in range(kc_n):
                        ins = PE.e.matmul(psb[banks[si]][:, :], lhsT=subs[si][:, kc, :], rhs=rhs(kc, tb),
                                          start=(kc == 0), stop=(kc == kc_n - 1))
                    rdy.append(PE.mark(ins))
                fr = epi(t, tb, banks, rdy)
                for bi, b in enumerate(banks):
                    bank_free[b] = fr[bi]
            wring.free[s] = rdy[-1]
            if side is not None:
                side()

    def wload_cols(wmat, ncols_list, kc_n):
        def f(t, slot):
            deps = []; subs = []
            cols = ncols_list(t)
            for i, c0 in enumerate(cols):
                sv = slot[:, i * kc_n * 128:(i + 1) * kc_n * 128].rearrange("p (kc n) -> p kc n", kc=kc_n)
                deps.append(POOL.dma(sv, wmat[:, c0:c0 + 128].rearrange("(kc p) n -> p kc n", p=128), wring.dsems[wring_slot_of(slot)]))
                subs.append(sv)
            return deps, subs
        return f

    slot_ids = {}
    for i in range(3):
        slot_ids[i] = wring.views[i]

    def wring_slot_of(slot):
        for i in range(3):
            if slot is wring.views[i]:
                return i
        raise KeyError

    NXS = 4
    xs_t = [k.sb(f"xs{i}", [128, 512], F32) for i in range(NXS)]
    xsring = Ring(k, "xs", [t[:] for t in xs_t])
    xs_store = [k.newsem(f"xst{i}") for i in range(NXS)]
    et_t = [k.sb(f"et{i}", [128, 512], F32) for i in range(2)]
    et_free = [None, None]
    et_i = [0]

    def make_res_epi(gvec, gbvec, tokbase, gate_b=None, out_dram=None):
        state = {}
        dst = xTs if out_dram is None else out_dram

        def pre(t, tb):
            s = xsring.next()
            SP.wait(xsring.free[s])
            state[(t, tb)] = (s, SP.dma(xsring.views[s], xTs[t * 128:(t + 1) * 128, tokbase + tb * 512: tokbase + (tb + 1) * 512],
                                        xsring.dsems[s]))

        def epi(t, tb, banks, rdy):
            s, xdep = state.pop((t, tb))
            b = banks[0]
            e = et_i[0] % 2
            et_i[0] += 1
            et = et_t[e]
            if gate_b is None:
                ACT.wait(rdy[0], et_free[e])
                if gbvec is None:
                    adep = ACT.mark(ACT.e.activation(out=et[:], in_=psb[b][:, :], func=AF.Identity, scale=gvec[:, t:t + 1]))
                else:
                    adep = ACT.mark(ACT.e.activation(out=et[:], in_=psb[b][:, :], func=AF.Identity, scale=gvec[:, t:t + 1],
                                                     bias=gbvec[:, t:t + 1]))
                DVE.wait(adep, xdep)
                ddep = DVE.mark(DVE.e.tensor_tensor(out=xs_t[s][:], in0=xs_t[s][:], in1=et[:], op=ALU.add))
                et_free[e] = ddep
                fr = adep
            else:
                DVE.wait(rdy[0], et_free[e], xdep)
                a1 = DVE.mark(DVE.e.tensor_tensor(out=et[:], in0=psb[b][:, :], in1=gate_b[:, tb * 512:(tb + 1) * 512], op=ALU.mult))
                DVE.wait(a1)
                ddep = DVE.mark(DVE.e.scalar_tensor_tensor(out=xs_t[s][:], in0=et[:], scalar=gvec[:, t:t + 1], in1=xs_t[s][:],
                                                           op0=ALU.mult, op1=ALU.add))
                et_free[e] = ddep
                fr = a1
            SP.wait(ddep)
            sdep = SP.dma(dst[t * 128:(t + 1) * 128, tokbase + tb * 512: tokbase + (tb + 1) * 512], xs_t[s][:], xs_store[s])
            xsring.free[s] = sdep
            return [fr]
        return pre, epi

    def finish_stores():
        for s in range(NXS):
            SP.wait((xs_store[s], xs_store[s].total))

    def ffn_half(ph, hT, wg, wu, wd, gvec, tokbase, aT, s_t, gate_b=None):
        s_free = [None, None]
        si = [0]

        def wload_gu(t, slot):
            sid = wring_slot_of(slot)
            svg = slot[:, 0:KC * 128].rearrange("p (kc n) -> p kc n", kc=KC)
            svu = slot[:, KC * 128:2 * KC * 128].rearrange("p (kc n) -> p kc n", kc=KC)
            d1 = POOL.dma(svg, wg[:, t * 128:(t + 1) * 128].rearrange("(kc p) n -> p kc n", p=128), wring.dsems[sid])
            d2 = POOL.dma(svu, wu[:, t * 128:(t + 1) * 128].rearrange("(kc p) n -> p kc n", p=128), wring.dsems[sid])
            return [d1, d2], [svg, svu]

        last_a = [None]

        def epi_gu(t, tb, banks, rdy):
            q = si[0] % 2
            si[0] += 1
            ACT.wait(rdy[0], s_free[q])
            sdep = ACT.mark(ACT.e.activation(out=s_t[q][:], in_=psb[banks[0]][:, :], func=AF.Silu))
            DVE.wait(sdep, rdy[1], ffn_state.get("aT_free"))
            adep = DVE.mark(DVE.e.tensor_tensor(out=aT[:, t, tb * 512:(tb + 1) * 512], in0=psb[banks[1]][:, :], in1=s_t[q][:], op=ALU.mult))
            s_free[q] = adep
            last_a[0] = adep
            return [sdep, adep]

        run_gemm(range(FC), 2, KC, wload_gu, lambda kc, tb: hT[:, kc, tb * 512:(tb + 1) * 512], range(2), epi_gu, side=mod_side)
        PE.wait(last_a[0])

        def wload_d(t, slot):
            sid = wring_slot_of(slot)
            sv = slot[:, 0:FC * 128].rearrange("p (kc n) -> p kc n", kc=FC)
            d1 = POOL.dma(sv, wd[:, t * 128:(t + 1) * 128].rearrange("(kc p) n -> p kc n", p=128), wring.dsems[sid])
            return [d1], [sv]

        pre, epi = make_res_epi(gvec, None, tokbase, gate_b=gate_b)
        run_gemm(range(KC), 1, FC, wload_d, lambda kc, tb: aT[:, kc, tb * 512:(tb + 1) * 512], range(2), epi, pre=pre, side=mod_side)
        ffn_state["aT_free"] = (PE.sem, PE.sem.total)

    ffn_state = {}

    def conv_layer(l):
        i = l
        with contextlib.ExitStack() as ph:
            hT = k.sb("hT", [128, KC, S], BF16, ph)
            with contextlib.ExitStack() as ph2:
                phase_norm(ph2, V(f"nmix{l}"), MOD(l, 0), MOD(l, 1), hT, 0, S)
                barrier()
            if stop_after == f"norm_{l}":
                POOL.e.wait_ge(bar.h, bar.total)
                POOL.dma(dbg_out[:, :].rearrange("(c p) t -> p c t", p=128), hT[:, :, :], misc)
                SP.wait((misc, misc.total))
                return True
            uT = k.sb("uT", [128, KC, S + 32], BF16, ph)
            phs = contextlib.ExitStack()
            DVE.e.memset(uT[:, :, 0:30], 0.0)
            sg_t = [k.sb(f"sg{q}", [128, 512], BF16, phs) for q in range(2)]
            sg_free = [None, None]
            sgi = [0]

            def wload1(t, slot):
                sid = wring_slot_of(slot)
                svv = slot[:, 0:KC * 128].rearrange("p (kc n) -> p kc n", kc=KC)
                svg = slot[:, KC * 128:2 * KC * 128].rearrange("p (kc n) -> p kc n", kc=KC)
                d1 = POOL.dma(svv, WT("conv_pw1_w")[i][:, t * 128:(t + 1) * 128].rearrange("(kc p) n -> p kc n", p=128), wring.dsems[sid])
                d2 = POOL.dma(svg, WT("conv_pw1_w")[i][:, D + t * 128: D + (t + 1) * 128].rearrange("(kc p) n -> p kc n", p=128), wring.dsems[sid])
                return [d1, d2], [svv, svg]

            bv = V(f"pw1bv{i}"); bg = V(f"pw1bg{i}")

            def epi1(t, tb, banks, rdy):
                q = sgi[0] % 2
                sgi[0] += 1
                ACT.wait(rdy[1], sg_free[q])
                sdep = ACT.mark(ACT.e.activation(out=sg_t[q][:], in_=psb[banks[1]][:, :], func=AF.Sigmoid, bias=bg[:, t:t + 1], scale=1.0))
                DVE.wait(sdep, rdy[0])
                udep = DVE.mark(DVE.e.scalar_tensor_tensor(out=uT[:, t, 30 + tb * 512: 30 + (tb + 1) * 512], in0=psb[banks[0]][:, :],
                                                           scalar=bv[:, t:t + 1], in1=sg_t[q][:], op0=ALU.add, op1=ALU.mult))
                sg_free[q] = udep
                return [udep, sdep]

            run_gemm(range(KC), 2, KC, wload1, lambda kc, tb: hT[:, kc, tb * 512:(tb + 1) * 512], range(4), epi1)
            barrier()
            if stop_after == f"pw1_{l}":
                POOL.e.wait_ge(bar.h, bar.total)
                POOL.dma(dbg_out[:, :].rearrange("(c p) t -> p c t", p=128), uT[:, :, 30:30 + S], misc)
                SP.wait((misc, misc.total))
                phs.close()
                return True
            phs.close(); phs = contextlib.ExitStack()
            vT = hT
            dww = k.sb("dww", [128, CW * 16], F32, phs)
            dwd = SP.dma(dww[:], dww_d[i], misc)
            diag = [k.sb(f"diag{q}", [128, CW, 128], BF16, phs) for q in range(2)]
            diag_free = [None, None]
            dwb = V(f"dwb{i}")
            DVE.wait(dwd)
            for j in range(KC):
                q = j % 2
                DVE.wait(diag_free[q])
                for kk in range(CW):
                    ins = DVE.e.tensor_scalar(out=diag[q][:, kk, :], in0=ident_b[:], scalar1=dww[:, kk * 16 + j: kk * 16 + j + 1],
                                              scalar2=0.0, op0=ALU.mult, op1=ALU.add)
                ddep = DVE.mark(ins)
                PE.wait(ddep)
                for tb in range(4):
                    b = bank_acquire()
                    for kk in range(CW):
                        ins = PE.e.matmul(psb[b][:, :], lhsT=diag[q][:, kk, :], rhs=uT[:, j, tb * 512 + kk: tb * 512 + kk + 512],
                                          start=(kk == 0), stop=(kk == CW - 1))
                    rdy = PE.mark(ins)
                    ACT.wait(rdy)
                    bank_free[b] = ACT.mark(ACT.e.activation(out=vT[:, j, tb * 512:(tb + 1) * 512], in_=psb[b][:, :], func=AF.Identity,
                                                             bias=dwb[:, j:j + 1], scale=1.0))
                diag_free[q] = rdy
            barrier()
            if stop_after == f"dw_{l}":
                POOL.e.wait_ge(bar.h, bar.total)
                POOL.dma(dbg_out[:, :].rearrange("(c p) t -> p c t", p=128), vT[:, :, :], misc)
                SP.wait((misc, misc.total))
                phs.close()
                return True
            phs.close(); phs = contextlib.ExitStack()
            sq = [k.sb(f"csq{q}", [128, 4, 512], BF16, phs) for q in range(2)]
            sq_free = [None, None]
            st = {n: k.sb(f"ln_{n}", [128, 512], F32, phs) for n in ["m", "msq", "var", "rstd"]}
            tmp = [k.sb(f"ctmp{q}", [128, 512], F32, phs) for q in range(3)]
            tmp_free = [None] * 3
            lng = V(f"lng{i}"); lnb = V(f"lnb{i}")
            ti = 0
            prev_tb_done = None
            for tb in range(4):
                b1 = bank_acquire(); b2 = bank_acquire()
                for g4 in range(4):
                    q = (tb * 4 + g4) % 2
                    ACT.wait(sq_free[q])
                    sdep = ACT.mark(ACT.e.activation(out=sq[q][:], in_=vT[:, g4 * 4:(g4 + 1) * 4, tb * 512:(tb + 1) * 512], func=AF.Square))
                    PE.wait(sdep)
                    for c4 in range(4):
                        c = g4 * 4 + c4
                        PE.e.matmul(psb[b1][:, :], lhsT=ones_b[:], rhs=vT[:, c, tb * 512:(tb + 1) * 512], start=(c == 0), stop=(c == KC - 1))
                        ins = PE.e.matmul(psb[b2][:, :], lhsT=ones_b[:], rhs=sq[q][:, c4, :], start=(c == 0), stop=(c == KC - 1))
                    sq_free[q] = PE.mark(ins)
                DVE.wait(sq_free[q], prev_tb_done)
                d1 = DVE.mark(DVE.e.tensor_scalar(out=st["m"][:], in0=psb[b1][:, :], scalar1=1.0 / D, scalar2=0.0, op0=ALU.mult, op1=ALU.add))
                DVE.wait(d1)
                d2 = DVE.mark(DVE.e.tensor_tensor(out=st["msq"][:], in0=st["m"][:], in1=st["m"][:], op=ALU.mult))
                DVE.wait(d2)
                d3 = DVE.mark(DVE.e.scalar_tensor_tensor(out=st["var"][:], in0=psb[b2][:, :], scalar=1.0 / D, in1=st["msq"][:],
                                                         op0=ALU.mult, op1=ALU.subtract))
                DVE.wait(d3)
                ACT.wait(d3)
                d4a = ACT.mark(ACT.e.activation(out=st["rstd"][:], in_=st["var"][:], func=AF.Sqrt, bias=eps_c[:, 0:1], scale=1.0))
                DVE.wait(d4a)
                d4 = DVE.mark(DVE.e.reciprocal(out=st["rstd"][:], in_=st["rstd"][:]))
                bank_free[b1] = d4; bank_free[b2] = d4
                DVE.wait(d4)
                for c in range(KC):
                    t = tmp[ti % 3]
                    DVE.wait(tmp_free[ti % 3])
                    e1 = DVE.mark(DVE.e.tensor_tensor(out=t[:], in0=vT[:, c, tb * 512:(tb + 1) * 512], in1=st["m"][:], op=ALU.subtract))
                    DVE.wait(e1)
                    e2 = DVE.mark(DVE.e.tensor_tensor(out=t[:], in0=t[:], in1=st["rstd"][:], op=ALU.mult))
                    ACT.wait(e2)
                    zdep = ACT.mark(ACT.e.activation(out=vT[:, c, tb * 512:(tb + 1) * 512], in_=t[:], func=AF.Silu,
                                                     scale=lng[:, c:c + 1], bias=lnb[:, c:c + 1]))
                    tmp_free[ti % 3] = zdep
                    ti += 1
                prev_tb_done = e2
            barrier()
            phs.close(); phs = contextlib.ExitStack()
            gb = k.sb("gb", [128, KC], F32, phs)
            gdep = DVE.mark(DVE.e.tensor_tensor(out=gb[:], in0=MOD(l, 2), in1=V(f"pw2b{i}"), op=ALU.mult))
            ACT.wait(gdep)

            def wload2(t, slot):
                sid = wring_slot_of(slot)
                sv = slot[:, 0:KC * 128].rearrange("p (kc n) -> p kc n", kc=KC)
                d1 = POOL.dma(sv, WT("conv_pw2_w")[i][:, t * 128:(t + 1) * 128].rearrange("(kc p) n -> p kc n", p=128), wring.dsems[sid])
                return [d1], [sv]

            pre, epi = make_res_epi(MOD(l, 2), gb, 0)
            run_gemm(range(KC), 1, KC, wload2, lambda kc, tb: vT[:, kc, tb * 512:(tb + 1) * 512], range(4), epi, pre=pre)
            finish_stores()
            barrier()
            phs.close()
        return False

    def ffn_layer(l):
        j = l // 2
        moe = (l % 2 == 1)
        if l == 0:
            nbanks[0] = 7
            mod_state["limit"] = mod_tiles_upto(2); mod_state["rate"] = 0.45; mod_state["acc"] = 0.0
        elif l == 1:
            mod_state["limit"] = mod_tiles_upto(5); mod_state["rate"] = 0.13; mod_state["acc"] = 0.0
        else:
            mod_state["rate"] = 0.0
        with contextlib.ExitStack() as ph:
            hTh = k.sb("hTh", [128, KC, 1024], BF16, ph)
            aT = k.sb("aT", [128, FC, 1024], BF16, ph)
            s_t = [k.sb(f"s{q}", [128, 512], BF16, ph) for q in range(2)]
            ffn_state.clear()
            if moe:
                rw = k.sb("rw", [128, KC, NE], F32, ph)
                rbias = k.sb("rbias", [NE, 1], F32, ph)
                rb0 = k.sb("rb0", [NE, 1], F32, ph)
                shf = k.sb("shf", [128, KC], F32, ph)
                lgT = k.sb("lgT", [NE, 1024], F32, ph)
                lg = k.sb("lg", [128, 8, NE], F32, ph)
                gates = k.sb("gates", [128, 8, NE], F32, ph)
                gT = k.sb("gT", [NE, 1024], BF16, ph)
                gate_b = [k.sb(f"gate_b{q}", [128, 1024], F32, ph) for q in range(2)]
                gate_free = [None, None]
                sel8 = [k.sb(f"sel8{q}", [NE, 128], BF16, ph) for q in range(2)]
                ones8 = k.sb("ones8", [NE, 128], F32, ph)
                sm = {n: k.sb(f"rt_{n}", [128, 8], F32, ph) for n in ["m1", "m2", "d", "e", "w1", "w2"]}
                big = {n: k.sb(f"rt_{n}", [128, 8, NE], F32, ph) for n in ["eq1", "l2", "eq2", "g1"]}
                d1 = SP.dma(rw[:], rw_d[j].rearrange("p (c e) -> p c e", e=NE), misc)
                d1 = SP.dma(rb0[:], rb_d[j], misc)
                DVE.wait(d1)
                DVE.e.memset(ones8[:], 1.0)
                PE.wait(d1)
                bb = bank_acquire()
                DVE.e.tensor_copy(out=shf[:], in_=MOD(l, 3))
                sdep = DVE.mark(DVE.e.tensor_copy(out=shf[:], in_=MOD(l, 3)))
                PE.wait(sdep)
                for c in range(KC):
                    ins = PE.e.matmul(psb[bb][0:NE, 0:1], lhsT=rw[:, c, :], rhs=shf[:, c:c + 1], start=(c == 0), stop=(c == KC - 1))
                pd = PE.mark(ins)
                DVE.wait(pd)
                bank_free[bb] = DVE.mark(DVE.e.tensor_tensor(out=rbias[:], in0=psb[bb][0:NE, 0:1], in1=rb0[:], op=ALU.add))
                ACT.wait(bank_free[bb])
            for half in range(2):
                tok0 = half * 1024
                with contextlib.ExitStack() as ph2:
                    router = dict(rw=rw, lgT=lgT, bias=rbias) if moe else None
                    phase_norm(ph2, V(f"nffn{l}"), MOD(l, 3), MOD(l, 4), hTh, tok0, 1024, router=router,
                               scratch=aT[:].rearrange("p f t -> p (f t)"))
                    barrier()
                if not moe:
                    ffn_half(ph, hTh, WT("ffn_w_gate")[j], WT("ffn_w_up")[j], WT("ffn_w_down")[j], MOD(l, 5), tok0, aT, s_t)
                else:
                    b = bank_acquire()
                    for tl in range(8):
                        ins = PE.e.transpose(out=psb[b][:, tl * NE:(tl + 1) * NE], in_=lgT[:, tl * 128:(tl + 1) * 128], identity=ident_f[0:NE, 0:NE])
                    pd = PE.mark(ins)
                    DVE.wait(pd)
                    c0 = DVE.mark(DVE.e.tensor_copy(out=lg[:], in_=psb[b][:, 0:8 * NE].rearrange("p (t e) -> p t e", e=NE)))
                    bank_free[b] = c0
                    DVE.wait(c0)

                    def bc(a):
                        return a[:, :].unsqueeze(2).broadcast_to([128, 8, NE])
                    x1 = DVE.mark(DVE.e.tensor_reduce(out=sm["m1"][:], in_=lg[:], axis=AX.X, op=ALU.max)); DVE.wait(x1)
                    x1 = DVE.mark(DVE.e.tensor_tensor(out=big["eq1"][:], in0=lg[:], in1=bc(sm["m1"]), op=ALU.is_equal)); DVE.wait(x1)
                    x1 = DVE.mark(DVE.e.scalar_tensor_tensor(out=big["l2"][:], in0=big["eq1"][:], scalar=-1e30, in1=lg[:], op0=ALU.mult, op1=ALU.add)); DVE.wait(x1)
                    x1 = DVE.mark(DVE.e.tensor_reduce(out=sm["m2"][:], in_=big["l2"][:], axis=AX.X, op=ALU.max)); DVE.wait(x1)
                    x1 = DVE.mark(DVE.e.tensor_tensor(out=big["eq2"][:], in0=big["l2"][:], in1=bc(sm["m2"]), op=ALU.is_equal)); DVE.wait(x1)
                    x1 = DVE.mark(DVE.e.tensor_tensor(out=sm["d"][:], in0=sm["m2"][:], in1=sm["m1"][:], op=ALU.subtract))
                    ACT.wait(x1)
                    x2 = ACT.mark(ACT.e.activation(out=sm["e"][:], in_=sm["d"][:], func=AF.Exp))
                    DVE.wait(x2)
                    x1 = DVE.mark(DVE.e.tensor_scalar(out=sm["w1"][:], in0=sm["e"][:], scalar1=1.0, scalar2=0.0, op0=ALU.add, op1=ALU.add)); DVE.wait(x1)
                    x1 = DVE.mark(DVE.e.reciprocal(out=sm["w1"][:], in_=sm["w1"][:])); DVE.wait(x1)
                    x1 = DVE.mark(DVE.e.tensor_tensor(out=sm["w2"][:], in0=sm["e"][:], in1=sm["w1"][:], op=ALU.mult)); DVE.wait(x1)
                    x1 = DVE.mark(DVE.e.tensor_tensor(out=big["g1"][:], in0=big["eq1"][:], in1=bc(sm["w1"]), op=ALU.mult)); DVE.wait(x1)
                    x1 = DVE.mark(DVE.e.tensor_tensor(out=gates[:], in0=big["eq2"][:], in1=bc(sm["w2"]), op=ALU.mult)); DVE.wait(x1)
                    x1 = DVE.mark(DVE.e.tensor_tensor(out=gates[:], in0=gates[:], in1=big["g1"][:], op=ALU.add))
                    PE.wait(x1)
                    bb2 = [bank_acquire(), bank_acquire()]
                    for tl in range(8):
                        ins = PE.e.transpose(out=psb[bb2[tl // 4]][0:NE, (tl % 4) * 128:(tl % 4 + 1) * 128], in_=gates[:, tl, :], identity=ident_f[:, :])
                    pd = PE.mark(ins)
                    DVE.wait(pd)
                    g0 = DVE.mark(DVE.e.tensor_copy(out=gT[:, 0:512], in_=psb[bb2[0]][0:NE, :]))
                    gtd = DVE.mark(DVE.e.tensor_copy(out=gT[:, 512:1024], in_=psb[bb2[1]][0:NE, :]))
                    bank_free[bb2[0]] = g0
                    bank_free[bb2[1]] = gtd
                    if stop_after == f"route_{l}" and half == 0:
                        POOL.wait(gtd)
                        POOL.dma(dbg_out[0:NE, 0:1024], gT[:], misc)
                        POOL.dma(dbg_out[NE:2 * NE, 0:1024], lgT[:], misc)
                        SP.wait((misc, misc.total))
                        return True
                    for e in range(NE):
                        q = e % 2
                        DVE.wait(gate_free[q], gtd)
                        sd = DVE.mark(DVE.e.tensor_scalar(out=sel8[q][:], in0=ones8[:], scalar1=ident_f[0:NE, e:e + 1], scalar2=0.0, op0=ALU.mult, op1=ALU.add))
                        PE.wait(sd)
                        gds = []
                        for tb in range(2):
                            b = bank_acquire()
                            pd = PE.mark(PE.e.matmul(psb[b][:, :], lhsT=sel8[q][:], rhs=gT[:, tb * 512:(tb + 1) * 512], start=True, stop=True))
                            ACT.wait(pd, gate_free[q])
                            gd = ACT.mark(ACT.e.activation(out=gate_b[q][:, tb * 512:(tb + 1) * 512], in_=psb[b][:, :], func=AF.Copy))
                            bank_free[b] = gd
                            gds.append(gd)
                        DVE.wait(gds)
                        ffn_half(ph, hTh, WT("moe_w_gate")[j, e], WT("moe_w_up")[j, e], WT("moe_w_down")[j, e], MOD(l, 5), tok0, aT, s_t, gate_b=gate_b[q])
                        gate_free[q] = [(DVE.sem, DVE.sem.total), (PE.sem, PE.sem.total)]
                if half == 1 and l == 0:
                    mod_flush(2)
                if half == 1 and l == 1:
                    mod_flush(5)
                    nbanks[0] = 8
                finish_stores()
                barrier()
        return False


    def kv_phase():
        with contextlib.ExitStack() as ph:
            hT = k.sb("hT", [128, KC, S], BF16, ph)
            with contextlib.ExitStack() as ph2:
                phase_norm(ph2, V("kvng"), KVMOD(0), KVMOD(1), hT, 0, S)
                barrier()
            wkvf = WT("w_kvf")
            stg = [k.sb(f"kst{q}", [128, 512], BF16, ph) for q in range(3)]
            stg_ring = Ring(k, "kst", [t[:] for t in stg], es=ph)
            cnt = [0]

            def evac(out, in_, waits):
                E = ACT if cnt[0] % 2 == 0 else DVE
                cnt[0] += 1
                E.wait(*waits)
                if E is ACT:
                    return ACT.mark(ACT.e.activation(out=out, in_=in_, func=AF.Copy))
                return DVE.mark(DVE.e.tensor_copy(out=out, in_=in_))

            def wloadk(t, slot):
                sid = wring_slot_of(slot)
                sv = slot[:, 0:KC * 128].rearrange("p (kc n) -> p kc n", kc=KC)
                d1 = POOL.dma(sv, wkvf[:, t * 128:(t + 1) * 128].rearrange("(kc p) n -> p kc n", p=128), wring.dsems[sid])
                return [d1], [sv]

            def epik(t, tb, banks, rdy):
                s = stg_ring.next()
                c = evac(stg[s][:], psb[banks[0]][:, :], [rdy[0], stg_ring.free[s]])
                SP.wait(c)
                stg_ring.free[s] = SP.dma(KTs[t * 128:(t + 1) * 128, tb * 512:(tb + 1) * 512], stg[s][:], stg_ring.dsems[s])
                return [c]

            run_gemm(range(H), 1, KC, wloadk, lambda kc, tb: hT[:, kc, tb * 512:(tb + 1) * 512], range(4), epik)
            vst = [k.sb(f"vst{q}", [128, 256], BF16, ph) for q in range(3)]
            vring = Ring(k, "vst", [t[:] for t in vst], es=ph)
            for cb in range(8):
                s = wring.next()
                POOL.wait(wring.free[s])
                sv = wring.views[s][:, 0:KC * 256].rearrange("p (kc n) -> p kc n", kc=KC)
                dep = POOL.dma(sv, wkvf[:, D + cb * 256: D + (cb + 1) * 256].rearrange("(kc p) n -> p kc n", p=128), wring.dsems[s])
                PE.wait(dep)
                for tt in range(16):
                    b = bank_acquire()
                    for kc in range(KC):
                        ins = PE.e.matmul(psb[b][:, 0:256], lhsT=hT[:, kc, tt * 128:(tt + 1) * 128], rhs=sv[:, kc, :],
                                          start=(kc == 0), stop=(kc == KC - 1))
                    rdy = PE.mark(ins)
                    q = vring.next()
                    c = evac(vst[q][:], psb[b][:, 0:256], [rdy, vring.free[q]])
                    bank_free[b] = c
                    SP.wait(c)
                    vring.free[q] = SP.dma(Vs[tt * 128:(tt + 1) * 128, cb * 256:(cb + 1) * 256], vst[q][:], vring.dsems[q])
                wring.free[s] = rdy
            wf = k.sb("wf", [128, KC, H], BF16, ph)
            dwf = POOL.dma(wf[:], wkvf[:, 2 * D:2 * D + H].rearrange("(kc p) n -> p kc n", p=128), misc)
            bft = k.sb("bft", [H, 1], F32, ph)
            nbf = k.sb("nbf", [H, 1], F32, ph)
            sp = k.sb("sp", [H, S], F32, ph)
            ones16 = k.sb("ones16", [H, S], F32, ph)
            PT = k.sb("PT", [H, S], F32, ph)
            d = SP.dma(bft[:], bf_d[:, :], misc)
            DVE.wait(d)
            DVE.e.memset(ones16[:], 1.0)
            nb = DVE.mark(DVE.e.tensor_scalar(out=nbf[:], in0=bft[:], scalar1=-1.0, scalar2=0.0, op0=ALU.mult, op1=ALU.add))
            PE.wait(dwf)
            for tb in range(4):
                b = bank_acquire()
                for kc in range(KC):
                    ins = PE.e.matmul(psb[b][0:H, :], lhsT=wf[:, kc, :], rhs=hT[:, kc, tb * 512:(tb + 1) * 512],
                                      start=(kc == 0), stop=(kc == KC - 1))
                rdy = PE.mark(ins)
                ACT.wait(rdy, nb)
                e1 = ACT.mark(ACT.e.activation(out=sp[:, tb * 512:(tb + 1) * 512], in_=psb[b][0:H, :], func=AF.Exp,
                                               bias=nbf[:, 0:1], scale=-1.0))
                bank_free[b] = e1
                ACT.wait(e1)
                e2 = ACT.mark(ACT.e.activation(out=sp[:, tb * 512:(tb + 1) * 512], in_=sp[:, tb * 512:(tb + 1) * 512], func=AF.Ln,
                                               bias=1.0, scale=1.0))
            DVE.wait(e2)
            scn = DVE.mark(DVE.e.tensor_tensor_scan(out=PT[:], data0=ones16[:], data1=sp[:], initial=0.0, op0=ALU.mult, op1=ALU.add))
            DVE.wait(scn)
            DVE.mark(DVE.e.tensor_scalar(out=GqT[:], in0=PT[:], scalar1=-float(np.sqrt(HD)), scalar2=0.0, op0=ALU.mult, op1=ALU.add))
            PE.wait(scn)
            b = bank_acquire()
            for tt in range(16):
                ins = PE.e.transpose(out=psb[b][:, tt * H:(tt + 1) * H], in_=PT[:, tt * 128:(tt + 1) * 128], identity=ident_f[0:H, 0:H])
            rdy = PE.mark(ins)
            DVE.wait(rdy)
            bank_free[b] = DVE.mark(DVE.e.tensor_copy(out=negF[:], in_=psb[b][:, 0:16 * H]))
            for r_ in (stg_ring, vring):
                for q in range(3):
                    SP.wait((r_.dsems[q], r_.dsems[q].total))
            if stop_after == "kv":
                POOL.e.wait_ge(bar.h, bar.total)
                barrier()
                POOL.e.wait_ge(bar.h, bar.total)
                POOL.dma(dbg_out[0:H, :], PT[:], misc)
                POOL.dma(dbg_out[128:256, 0:256], negF[:], misc)
                POOL.dma(dbg_out[256:256 + H, :], GqT[:], misc)
                SP.wait((misc, misc.total))
                return True
            barrier()
        return False

    def attn_layer(l):
        i = l - 2
        with contextlib.ExitStack() as ph:
            hT = k.sb("hT", [128, KC, S], BF16, ph)
            with contextlib.ExitStack() as ph2:
                phase_norm(ph2, V(f"nmix{l}"), MOD(l, 0), MOD(l, 1), hT, 0, S)
                barrier()
            qT = k.sb("qT", [128, H, S], BF16, ph)
            cnt = [0]

            def wloadq(t, slot):
                sid = wring_slot_of(slot)
                sv = slot[:, 0:KC * 128].rearrange("p (kc n) -> p kc n", kc=KC)
                d1 = POOL.dma(sv, WT("attn_wq")[i][:, t * 128:(t + 1) * 128].rearrange("(kc p) n -> p kc n", p=128), wring.dsems[sid])
                return [d1], [sv]

            def epiq(t, tb, banks, rdy):
                E = ACT if cnt[0] % 2 == 0 else DVE
                cnt[0] += 1
                E.wait(rdy[0])
                if E is ACT:
                    c = ACT.mark(ACT.e.activation(out=qT[:, t, tb * 512:(tb + 1) * 512], in_=psb[banks[0]][:, :], func=AF.Copy))
                else:
                    c = DVE.mark(DVE.e.tensor_copy(out=qT[:, t, tb * 512:(tb + 1) * 512], in_=psb[banks[0]][:, :]))
                return [c]

            run_gemm(range(H), 1, KC, wloadq, lambda kc, tb: hT[:, kc, tb * 512:(tb + 1) * 512], range(4), epiq)
            barrier()
            oT = hT
            phs = contextlib.ExitStack()
            KT = [k.sb(f"KT{q}", [128, S], BF16, phs) for q in range(2)]
            Vh = [k.sb(f"Vh{q}", [128, 16, 128], BF16, phs) for q in range(2)]
            kvring = Ring(k, "kv", [None, None], es=phs)
            pT = [k.sb(f"pT{q}", [128, 512], BF16, phs) for q in range(4)]
            pT_free = [None] * 4
            pi = [0]
            selh = [k.sb(f"selh{q}", [H, 128], BF16, phs) for q in range(2)]
            sel_free = [None, None]
            ones16b = k.sb("ones16b", [H, 128], F32, phs)
            rd = [k.sb(f"rd{q}", [128, 512], F32, phs) for q in range(1)]
            rd_free = [None]
            ri = [0]
            o1 = DVE.mark(DVE.e.memset(ones16b[:], 1.0))
            DVE.wait(o1)
            scale = float(HD ** -0.5)
            acc_i = [0]; sc_i = [0]
            for h in range(H):
                s = kvring.next()
                SP.wait(kvring.free[s])
                d1 = SP.dma(KT[s][:], KTs[h * 128:(h + 1) * 128, :], kvring.dsems[s])
                d2 = SP.dma(Vh[s][:], Vs[:, h * 128:(h + 1) * 128].rearrange("(i p) d -> p i d", p=128), kvring.dsems[s])
                q2 = h % 2
                DVE.wait(sel_free[q2])
                sd = DVE.mark(DVE.e.tensor_scalar(out=selh[q2][:], in0=ones16b[:], scalar1=ident_f[0:H, h:h + 1], scalar2=0.0,
                                                  op0=ALU.mult, op1=ALU.add))
                PE.wait(d1, d2, sd)
                fin = None
                for j in range(4):
                    acc_i[0] += 1
                    bo, bd = (0, 1) if acc_i[0] % 2 == 0 else (2, 3)
                    PE.wait(bank_free[bo], bank_free[bd])
                    ntile = 4 * (j + 1)

                    def qk(ii):
                        o = max(0, ii - 4 * j)
                        c0 = 128 * o
                        sc_i[0] += 1
                        b = 4 + sc_i[0] % 4
                        PE.wait(bank_free[b])
                        PE.e.matmul(psb[b][:, c0:512], lhsT=KT[s][:, ii * 128:(ii + 1) * 128], rhs=qT[:, h, j * 512 + c0:(j + 1) * 512],
                                    start=True, stop=False)
                        ins = PE.e.matmul(psb[b][:, c0:512], lhsT=selh[q2][:], rhs=GqT[:, j * 512 + c0:(j + 1) * 512], start=False, stop=True)
                        return b, c0, PE.mark(ins)

                    pend = []
                    nxt = 0
                    while nxt < min(3, ntile):
                        pend.append(qk(nxt))
                        nxt += 1
                    for ii in range(ntile):
                        b, c0, rdy = pend.pop(0)
                        if nxt < ntile:
                            pend.append(qk(nxt))
                            nxt += 1
                        p = pi[0] % 4
                        pi[0] += 1
                        ACT.wait(rdy, pT_free[p])
                        e = ACT.mark(ACT.e.activation(out=pT[p][:, c0:512], in_=psb[b][:, c0:512], func=AF.Exp,
                                                      bias=negF[:, ii * H + h: ii * H + h + 1], scale=scale))
                        bank_free[b] = e
                        dep = e
                        if ii >= 4 * j:
                            DVE.wait(e)
                            dep = DVE.mark(DVE.e.tensor_tensor(out=pT[p][:, c0:c0 + 128], in0=pT[p][:, c0:c0 + 128], in1=tri_b[:], op=ALU.mult))
                        PE.wait(dep)
                        PE.e.matmul(psb[bo][:, c0:512], lhsT=Vh[s][:, ii, :], rhs=pT[p][:, c0:512], start=(ii == 0), stop=(ii == ntile - 1))
                        ins = PE.e.matmul(psb[bd][:, c0:512], lhsT=ones_b[:], rhs=pT[p][:, c0:512], start=(ii == 0), stop=(ii == ntile - 1))
                        pT_free[p] = PE.mark(ins)
                    fin = pT_free[p]
                    r = 0
                    DVE.wait(fin, rd_free[r])
                    r1 = DVE.mark(DVE.e.reciprocal(out=rd[r][:], in_=psb[bd][:, :]))
                    DVE.wait(r1)
                    r2 = DVE.mark(DVE.e.tensor_tensor(out=oT[:, h, j * 512:(j + 1) * 512], in0=psb[bo][:, :], in1=rd[r][:], op=ALU.mult))
                    rd_free[r] = r2
                    bank_free[bo] = r2
                    bank_free[bd] = r1
                kvring.free[s] = fin
                sel_free[q2] = fin
            barrier()
            phs.close()
            if stop_after == f"att_{l}":
                POOL.e.wait_ge(bar.h, bar.total)
                POOL.dma(dbg_out[:, :].rearrange("(c p) t -> p c t", p=128), oT[:, :, :], misc)
                SP.wait((misc, misc.total))
                return True

            def wloado(t, slot):
                sid = wring_slot_of(slot)
                sv = slot[:, 0:KC * 128].rearrange("p (kc n) -> p kc n", kc=KC)
                d1 = POOL.dma(sv, WT("attn_wo")[i][:, t * 128:(t + 1) * 128].rearrange("(kc p) n -> p kc n", p=128), wring.dsems[sid])
                return [d1], [sv]

            pre, epi = make_res_epi(MOD(l, 2), None, 0)
            run_gemm(range(KC), 1, KC, wloado, lambda kc, tb: oT[:, kc, tb * 512:(tb + 1) * 512], range(4), epi, pre=pre)
            finish_stores()
            barrier()
        return False

    def final_phase(outT):
        with contextlib.ExitStack() as ph:
            zer = k.sb("zer", [128, KC], F32, ph)
            z0 = DVE.mark(DVE.e.memset(zer[:], 0.0))
            DVE.wait(z0)
            last = phase_norm(ph, V("fng"), None, zer[:], None, 0, S, out_dram=outT)
            SP.wait(last)
            for q in range(3):
                pass
        return last

    def run_seq():
        outT = outT_all[cur[0]]
        seq_setup()
        phase_mod()
        barrier()
        if stop_after == "mod":
            SP.dma(dbg_out[:, 0:416], modall[:, :], misc)
            SP.wait((misc, misc.total))
            return True
        done = False
        if stop_after == "only_attn":
            assert not kv_phase()
            assert not attn_layer(2)
            finish_stores()
            barrier()
            d = SP.dma(outT, xTs[:, :], misc)
            SP.wait(d)
            return True
        for l in range(4):
            if l == 2:
                if kv_phase():
                    return True
            r_ = conv_layer(l) if l < 2 else attn_layer(l)
            if r_:
                return True
            if stop_after == f"mix_{l}":
                done = True
                break
            if ffn_layer(l):
                return True
            if stop_after == f"ffn_{l}":
                done = True
                break
        finish_stores()
        barrier()
        if done:
            d = SP.dma(outT, xTs[:, :], misc)
            SP.wait(d)
            return True
        final_phase(outT)
        barrier()
        return False

    for si in range(nseq):
        cur[0] = si
        if run_seq():
            return


def _prep_common(inp):
    g = lambda n: np.asarray(inp[n], np.float32)
    dww = np.zeros((2, 128, CW * 16), np.float32)
    for i in range(2):
        for kk in range(CW):
            dww[i][:, kk * 16:(kk + 1) * 16] = _fm(g("conv_dw_w")[i, kk])
    rw = np.zeros((2, 128, KC * NE), np.float32)
    for j in range(2):
        rw[j] = g("moe_router_w")[j].reshape(KC, 128, NE).transpose(1, 0, 2).reshape(128, KC * NE)
    rb = g("moe_router_b").reshape(2, NE, 1).copy()
    common = dict(
        dww=dww, bf=g("b_f").reshape(H, 1).copy(), rw=rw, rb=rb,
        ident=np.eye(128, dtype=np.float32), tri=np.triu(np.ones((128, 128), np.float32)),
        ada_w=g("ada_w"), conv_pw1_w=g("conv_pw1_w"), conv_pw2_w=g("conv_pw2_w"), kv_ada_w=g("kv_ada_w"),
        w_kvf=g("w_kvf"), attn_wq=g("attn_wq"), attn_wo=g("attn_wo"), ffn_w_gate=g("ffn_w_gate"),
        ffn_w_up=g("ffn_w_up"), ffn_w_down=g("ffn_w_down"), moe_w_gate=g("moe_w_gate"),
        moe_w_up=g("moe_w_up"), moe_w_down=g("moe_w_down"),
    )
    return common


def _vecs_for(inp, b):
    g = lambda n: np.asarray(inp[n], np.float32)
    v = np.zeros((128, NV * 16), np.float32)

    def put(name, vec):
        i = VIDX[name]
        v[:, i * 16:(i + 1) * 16] = _fm(vec)
    put("c", g("c")[b])
    for l in range(4):
        for s in range(6):
            put(f"ab{l}_{s}", g("ada_b")[l, s * D:(s + 1) * D])
        put(f"nmix{l}", g("norm_mix_g")[l]); put(f"nffn{l}", g("norm_ffn_g")[l])
    put("kvab_0", g("kv_ada_b")[:D]); put("kvab_1", g("kv_ada_b")[D:])
    for i in range(2):
        put(f"pw1bv{i}", g("conv_pw1_b")[i, :D]); put(f"pw1bg{i}", g("conv_pw1_b")[i, D:])
        put(f"dwb{i}", g("conv_dw_b")[i]); put(f"lng{i}", g("conv_ln_g")[i]); put(f"lnb{i}", g("conv_ln_b")[i])
        put(f"pw2b{i}", g("conv_pw2_b")[i])
    put("kvng", g("kv_norm_g")); put("fng", g("final_norm_g"))
    return v


def make_in_maps(inp, seqs_per_core, names=None):
    common = _prep_common(inp)
    if names is not None:
        common = {k_: v_ for k_, v_ in common.items() if k_ in names}
    x = np.asarray(inp["x"], np.float32)
    maps = []
    for seqs in seqs_per_core:
        m = dict(common)
        m["xT"] = np.stack([np.ascontiguousarray(x[b].T) for b in seqs], 0)
        m["vecs"] = np.stack([_vecs_for(inp, b) for b in seqs], 0)
        maps.append(m)
    return maps


NUSED = 8
NSEQ = 8 // NUSED


def kernel(**inputs):
    nc = build(nseq=NSEQ)
    seqs = [list(range(c * NSEQ, (c + 1) * NSEQ)) for c in range(NUSED)]
    in_maps = make_in_maps(inputs, seqs)
    res = run_bass_kernel_spmd(nc, in_maps, core_ids=list(range(NUSED)))
    out = np.zeros((8, S, D), np.float32)
    for c in range(NUSED):
        o = np.asarray(res.results[c]["outT"])
        for i, b in enumerate(seqs[c]):
            out[b] = o[i].T
    return out
```
